# Optimizing a Trainium2 kernel written in Bass

```python
import jax, jax.numpy as jnp
from jax import lax
import numpy as np

D_MODEL = 1024
BATCH = 2
SEQ = 8192
DEPTH = 2

HEAD_DIM = 64
POOL_WINDOWS = (2, 4, 8, 16)
POOL_WIDTH = D_MODEL // 4
POOL_GROUP = POOL_WIDTH // len(POOL_WINDOWS)
RWKV_WIDTH = 3 * D_MODEL // 8
RWKV_HEADS = RWKV_WIDTH // HEAD_DIM
DECAY_LORA = 64
ICLR_LORA = 64
GATE_LORA = 128
VRES_LORA = 32
RWKV_GN_EPS = 64e-5
RWKV_COLS = 3 * RWKV_WIDTH + DECAY_LORA + ICLR_LORA + GATE_LORA
MLSTM_WIDTH = D_MODEL - POOL_WIDTH - RWKV_WIDTH
MLSTM_HEADS = MLSTM_WIDTH // HEAD_DIM
MLSTM_CONV = 4
MLSTM_CHUNK = 64
MLSTM_COLS = 4 * MLSTM_WIDTH + 2 * MLSTM_HEADS
D_MIX = POOL_WIDTH + RWKV_WIDTH + MLSTM_WIDTH
D_IN = POOL_WIDTH + RWKV_COLS + MLSTM_COLS
N_EXPERTS = 32
TOP_K = 4
D_FF = D_MODEL
SWIGLU_LIMIT = 7.0
SWIGLU_ALPHA = 1.702
MOE_BLOCK = 256
LN_EPS = 1e-5
DEEPNORM_ALPHA = (2 * DEPTH) ** 0.25
DEEPNORM_BETA = (8 * DEPTH) ** -0.25

kernel_name = 'hybrid_pool_rwkv7_mlstm_moe_deepnorm'

F32 = jnp.float32


def layer_norm(x, g, b):
    xf = x.astype(F32)
    mu = xf.mean(-1, keepdims=True)
    var = jnp.square(xf - mu).mean(-1, keepdims=True)
    return ((xf - mu) * lax.rsqrt(var + LN_EPS) * g + b).astype(x.dtype)


def token_shift(x):
    return jnp.pad(x, ((0, 0), (1, 0), (0, 0)))[:, :x.shape[1]]


def causal_depthwise_conv(x, w):
    k_taps, s = w.shape[0], x.shape[1]
    xp = jnp.pad(x, ((0, 0), (k_taps - 1, 0), (0, 0)))
    return sum(xp[:, i:i + s] * w[i] for i in range(k_taps))


def pool_mixer(u, pool_w, pool_scale):
    b, s, _ = u.shape
    uf = u.astype(F32)
    cs = jnp.pad(jnp.cumsum(uf, axis=1), ((0, 0), (1, 0), (0, 0)))
    t = jnp.arange(s)
    outs = []
    for gi, win in enumerate(POOL_WINDOWS):
        sl = slice(gi * POOL_GROUP, (gi + 1) * POOL_GROUP)
        lo = jnp.maximum(t + 1 - win, 0)
        cnt = jnp.minimum(t + 1, win).astype(F32)
        outs.append((cs[:, 1:, sl] - cs[:, lo, sl]) / cnt[None, :, None] - uf[..., sl])
    d = jnp.stack(outs, axis=2)
    y = jnp.einsum('bsgc,gcd->bsgd', d, pool_w.astype(F32)).reshape(b, s, POOL_WIDTH)
    return (y * pool_scale).astype(u.dtype)


def rwkv7_recurrence(r, decay, k, v, a_vec, b_vec):
    b, s, h, n = r.shape

    def step(state, inp):
        r_t, w_t, k_t, v_t, a_t, b_t = inp
        sa = jnp.einsum('bhvk,bhk->bhv', state, a_t)
        state = state * w_t[:, :, None, :] + sa[..., None] * b_t[:, :, None, :] + v_t[..., None] * k_t[:, :, None, :]
        return state, jnp.einsum('bhvk,bhk->bhv', state, r_t)

    xs = tuple(jnp.moveaxis(z, 1, 0) for z in (r, decay, k, v, a_vec, b_vec))
    _, ys = lax.scan(step, jnp.zeros((b, h, n, n), F32), xs)
    return jnp.moveaxis(ys, 0, 1)


def rwkv7_mixer(p, mu, w0, w2, a0, a2, g2, kk_scale, ka, rk, lnx_g, lnx_b, v_first, v_gate):
    b, s, _ = p.shape
    pf = p.astype(F32)
    pf = pf + mu * (token_shift(pf) - pf)
    W = RWKV_WIDTH
    r, k, v, wd, ad, gd = jnp.split(pf, [W, 2 * W, 3 * W, 3 * W + DECAY_LORA, 3 * W + DECAY_LORA + ICLR_LORA], axis=-1)
    w_log = -jax.nn.softplus(-(w0 + jnp.tanh(wd) @ w2)) - 0.5
    decay = jnp.exp(-jnp.exp(w_log))
    a = jax.nn.sigmoid(a0 + ad @ a2)
    g = jax.nn.sigmoid(gd) @ g2
    if v_first is None:
        v_first = v
    else:
        v = v + (v_first - v) * v_gate

    def heads(z):
        return z.reshape(b, s, RWKV_HEADS, HEAD_DIM)

    kk = heads(k * kk_scale)
    kk = kk / jnp.maximum(jnp.sqrt(jnp.sum(kk * kk, -1, keepdims=True)), 1e-12)
    k = k * (1.0 + (a - 1.0) * ka)
    rh, kh, vh = heads(r), heads(k), heads(v)
    y = rwkv7_recurrence(rh, heads(decay), kh, vh, -kk, kk * heads(a))
    ym = y.mean(-1, keepdims=True)
    yv = jnp.square(y - ym).mean(-1, keepdims=True)
    y = ((y - ym) * lax.rsqrt(yv + RWKV_GN_EPS)).reshape(b, s, W) * lnx_g + lnx_b
    bonus = jnp.sum(rh * kh * rk, -1, keepdims=True) * vh
    y = y + bonus.reshape(b, s, W)
    return (y * g).astype(p.dtype), v_first


def mlstm_mixer(p, conv_w, conv_b, b_i, b_f, norm_g):
    b, s, _ = p.shape
    H, Dh, L = MLSTM_HEADS, HEAD_DIM, MLSTM_CHUNK
    nc = s // L
    W = MLSTM_WIDTH
    pf = p.astype(F32)
    qk, v, o, ig, fg = jnp.split(pf, [2 * W, 3 * W, 4 * W, 4 * W + H], axis=-1)
    qk = jax.nn.silu(causal_depthwise_conv(qk, conv_w) + conv_b)
    q, k = qk[..., :W], qk[..., W:]

    def chunks(z):
        return z.reshape(b, nc, L, H, Dh).transpose(0, 3, 1, 2, 4)

    def gchunks(z):
        return z.reshape(b, nc, L, H).transpose(0, 3, 1, 2)

    q = chunks(q) * (Dh ** -0.5)
    k = chunks(k)
    v = chunks(v)
    ig = gchunks(ig + b_i)
    g = jnp.cumsum(jax.nn.log_sigmoid(gchunks(fg + b_f)), axis=-1)
    g_last = g[..., -1]
    e = g_last[..., None] - g + ig
    m_loc = e.max(-1)
    w_e = jnp.exp(e - m_loc[..., None])
    kv_loc = jnp.einsum('bhcs,bhcsk,bhcsv->bhckv', w_e, k, v)
    n_loc = jnp.einsum('bhcs,bhcsk->bhck', w_e, k)

    def step(carry, inp):
        c_st, n_st, m_st = carry
        gl, ml, kvl, nl = inp
        m_new = jnp.maximum(gl + m_st, ml)
        a_old = jnp.exp(gl + m_st - m_new)
        a_new = jnp.exp(ml - m_new)
        c_new = a_old[..., None, None] * c_st + a_new[..., None, None] * kvl
        n_new = a_old[..., None] * n_st + a_new[..., None] * nl
        return (c_new, n_new, m_new), (c_st, n_st, m_st)

    init = (jnp.zeros((b, H, Dh, Dh), F32), jnp.zeros((b, H, Dh), F32), jnp.zeros((b, H), F32))
    xs = (jnp.moveaxis(g_last, 2, 0), jnp.moveaxis(m_loc, 2, 0), jnp.moveaxis(kv_loc, 2, 0), jnp.moveaxis(n_loc, 2, 0))
    _, (c_prev, n_prev, m_prev) = lax.scan(step, init, xs)
    c_prev = jnp.moveaxis(c_prev, 0, 2)
    n_prev = jnp.moveaxis(n_prev, 0, 2)
    m_prev = jnp.moveaxis(m_prev, 0, 2)
    causal = jnp.tril(jnp.ones((L, L), bool))
    d_log = jnp.where(causal, g[..., :, None] - g[..., None, :] + ig[..., None, :], -jnp.inf)
    inter_log = g + m_prev[..., None]
    m_j = jnp.maximum(d_log.max(-1), inter_log)
    w_intra = jnp.exp(d_log - m_j[..., None]) * jnp.einsum('bhcjd,bhcsd->bhcjs', q, k)
    w_inter = jnp.exp(inter_log - m_j)
    num = jnp.einsum('bhcjs,bhcsv->bhcjv', w_intra, v) + w_inter[..., None] * jnp.einsum('bhcjd,bhcdv->bhcjv', q, c_prev)
    den = w_intra.sum(-1) + w_inter * jnp.einsum('bhcjd,bhcd->bhcj', q, n_prev)
    h = num / jnp.maximum(jnp.abs(den), jnp.exp(-m_j))[..., None]
    h = h.transpose(0, 2, 3, 1, 4).reshape(b, s, H, Dh)
    hm = h.mean(-1, keepdims=True)
    hv = jnp.square(h - hm).mean(-1, keepdims=True)
    hn = ((h - hm) * lax.rsqrt(hv + LN_EPS)).reshape(b, s, W) * norm_g
    return (jax.nn.sigmoid(o) * hn).astype(p.dtype)


def moe_ffn(h, router_w, router_b, w_gu, b_gu, w_dn, b_dn):
    bsz, s, d = h.shape
    xt = h.reshape(-1, d)
    t = xt.shape[0]
    logits = (xt @ router_w + router_b).astype(F32)
    top_val, top_idx = lax.top_k(logits, TOP_K)
    gate = jax.nn.softmax(top_val, axis=-1)
    flat_e = top_idx.reshape(-1)
    order = jnp.argsort(flat_e)
    e_sorted = flat_e[order]
    tok_sorted = order // TOP_K
    counts = jnp.bincount(flat_e, length=N_EXPERTS)
    padded = (counts + MOE_BLOCK - 1) // MOE_BLOCK * MOE_BLOCK
    pad_end = jnp.cumsum(padded)
    pad_start = pad_end - padded
    start = jnp.cumsum(counts) - counts
    dest = pad_start[e_sorted] + jnp.arange(t * TOP_K) - start[e_sorted]
    n_blocks = -(-(t * TOP_K) // MOE_BLOCK) + N_EXPERTS
    buf = jnp.zeros((n_blocks * MOE_BLOCK, d), h.dtype).at[dest].set(xt[tok_sorted])
    block_e = jnp.minimum(jnp.searchsorted(pad_end, jnp.arange(n_blocks) * MOE_BLOCK, side='right'), N_EXPERTS - 1)

    def expert_block(args):
        xb, e = args
        gu = xb @ w_gu[e] + b_gu[e]
        glu = jnp.minimum(gu[:, :D_FF], SWIGLU_LIMIT)
        lin = jnp.clip(gu[:, D_FF:], -SWIGLU_LIMIT, SWIGLU_LIMIT)
        act = glu * jax.nn.sigmoid(SWIGLU_ALPHA * glu) * (lin + 1.0)
        return act @ w_dn[e] + b_dn[e]

    out = lax.map(expert_block, (buf.reshape(n_blocks, MOE_BLOCK, d), block_e)).reshape(-1, d)
    y_sorted = out[dest] * gate.reshape(-1)[order][:, None].astype(h.dtype)
    y = jnp.zeros_like(xt).at[tok_sorted].add(y_sorted)
    return y.reshape(bsz, s, d)


def setup_inputs(seed: int = 0) -> dict:
    key = jax.random.key(seed)
    ks = iter(jax.random.split(key, 48))
    Lr = DEPTH

    def nrm(shape, scale):
        return jax.random.normal(next(ks), shape, F32) * scale

    W, H = RWKV_WIDTH, RWKV_HEADS
    return {
        'x': nrm((BATCH, SEQ, D_MODEL), 1.0),
        'w_in': nrm((Lr, D_MODEL, D_IN), D_MODEL ** -0.5),
        'pool_w': nrm((Lr, len(POOL_WINDOWS), POOL_GROUP, POOL_GROUP), POOL_GROUP ** -0.5),
        'pool_scale': 1.0 + nrm((Lr, POOL_WIDTH), 0.1),
        'rwkv_mu': jax.random.uniform(next(ks), (Lr, RWKV_COLS), F32),
        'rwkv_w0': jnp.linspace(-6.0, -1.0, W, dtype=F32)[None] + nrm((Lr, W), 0.1),
        'rwkv_w2': nrm((Lr, DECAY_LORA, W), 0.1),
        'rwkv_a0': nrm((Lr, W), 0.1),
        'rwkv_a2': nrm((Lr, ICLR_LORA, W), 0.1),
        'rwkv_g2': nrm((Lr, GATE_LORA, W), GATE_LORA ** -0.5),
        'rwkv_kk_scale': 0.85 + nrm((Lr, W), 0.05),
        'rwkv_ka': 1.0 + nrm((Lr, W), 0.05),
        'rwkv_rk': nrm((Lr, H, HEAD_DIM), 0.1),
        'rwkv_lnx_g': 1.0 + nrm((Lr, W), 0.1),
        'rwkv_lnx_b': nrm((Lr, W), 0.01),
        'rwkv_v0': 1.0 + nrm((Lr - 1, W), 0.1),
        'rwkv_v1': nrm((Lr - 1, D_MODEL, VRES_LORA), D_MODEL ** -0.5),
        'rwkv_v2': nrm((Lr - 1, VRES_LORA, W), VRES_LORA ** -0.5),
        'mlstm_conv_w': nrm((Lr, MLSTM_CONV, 2 * MLSTM_WIDTH), MLSTM_CONV ** -0.5),
        'mlstm_conv_b': nrm((Lr, 2 * MLSTM_WIDTH), 0.01),
        'mlstm_b_i': nrm((Lr, MLSTM_HEADS), 0.1),
        'mlstm_b_f': jnp.linspace(3.0, 6.0, MLSTM_HEADS, dtype=F32)[None] + nrm((Lr, MLSTM_HEADS), 0.1),
        'mlstm_norm_g': 1.0 + nrm((Lr, MLSTM_WIDTH), 0.1),
        'w_out': nrm((Lr, D_MIX, D_MODEL), D_MIX ** -0.5 * DEEPNORM_BETA),
        'ln1_g': 1.0 + nrm((Lr, D_MODEL), 0.1),
        'ln1_b': nrm((Lr, D_MODEL), 0.01),
        'router_w': nrm((Lr, D_MODEL, N_EXPERTS), D_MODEL ** -0.5),
        'router_b': nrm((Lr, N_EXPERTS), 0.01),
        'w_gate_up': nrm((Lr, N_EXPERTS, D_MODEL, 2 * D_FF), D_MODEL ** -0.5),
        'b_gate_up': nrm((Lr, N_EXPERTS, 2 * D_FF), 0.01),
        'w_down': nrm((Lr, N_EXPERTS, D_FF, D_MODEL), D_FF ** -0.5 * DEEPNORM_BETA),
        'b_down': nrm((Lr, N_EXPERTS, D_MODEL), 0.01),
        'ln2_g': 1.0 + nrm((Lr, D_MODEL), 0.1),
        'ln2_b': nrm((Lr, D_MODEL), 0.01),
    }


def reference(x, w_in, pool_w, pool_scale, rwkv_mu, rwkv_w0, rwkv_w2, rwkv_a0, rwkv_a2, rwkv_g2,
              rwkv_kk_scale, rwkv_ka, rwkv_rk, rwkv_lnx_g, rwkv_lnx_b, rwkv_v0, rwkv_v1, rwkv_v2,
              mlstm_conv_w, mlstm_conv_b, mlstm_b_i, mlstm_b_f, mlstm_norm_g, w_out, ln1_g, ln1_b,
              router_w, router_b, w_gate_up, b_gate_up, w_down, b_down, ln2_g, ln2_b):
    h = x
    v_first = None
    for l in range(DEPTH):
        p = h @ w_in[l]
        p_pool = p[..., :POOL_WIDTH]
        p_rwkv = p[..., POOL_WIDTH:POOL_WIDTH + RWKV_COLS]
        p_mlstm = p[..., POOL_WIDTH + RWKV_COLS:]
        y_pool = pool_mixer(p_pool, pool_w[l], pool_scale[l])
        if l == 0:
            v_gate = None
        else:
            v_gate = jax.nn.sigmoid(rwkv_v0[l - 1] + (h @ rwkv_v1[l - 1]) @ rwkv_v2[l - 1]).astype(F32)
        y_rwkv, v_first = rwkv7_mixer(p_rwkv, rwkv_mu[l], rwkv_w0[l], rwkv_w2[l], rwkv_a0[l], rwkv_a2[l],
                                      rwkv_g2[l], rwkv_kk_scale[l], rwkv_ka[l], rwkv_rk[l],
                                      rwkv_lnx_g[l], rwkv_lnx_b[l], v_first, v_gate)
        y_mlstm = mlstm_mixer(p_mlstm, mlstm_conv_w[l], mlstm_conv_b[l], mlstm_b_i[l], mlstm_b_f[l], mlstm_norm_g[l])
        mix = jnp.concatenate([y_pool, y_rwkv, y_mlstm], axis=-1) @ w_out[l]
        h = layer_norm(DEEPNORM_ALPHA * h + mix, ln1_g[l], ln1_b[l])
        ffn = moe_ffn(h, router_w[l], router_b[l], w_gate_up[l], b_gate_up[l], w_down[l], b_down[l])
        h = layer_norm(DEEPNORM_ALPHA * h + ffn, ln2_g[l], ln2_b[l])
    return h
```

```python
import numpy as np
import concourse.bass as bass
import concourse.mybir as mybir
from concourse.bass_utils import run_bass_kernel_spmd

F32 = mybir.dt.float32
BF16 = mybir.dt.bfloat16
AF = mybir.ActivationFunctionType
ALU = mybir.AluOpType
AX = mybir.AxisListType

D = 1024
NE = 32
ALPHA = 4.0 ** 0.25
LN_EPS = 1e-5
NCORES = 8

ENG = ("pe", "act", "dve", "pool", "sp")


class Buf:
    __slots__ = ("w", "r")

    def __init__(self):
        self.w = None
        self.r = []


class Op:
    __slots__ = ("eng", "fn", "deps", "inc", "dma", "tok")

    def __init__(self, eng, fn, dma):
        self.eng = eng
        self.fn = fn
        self.deps = []
        self.inc = False
        self.dma = dma
        self.tok = None


class Sched:
    def __init__(self, n_dma=20):
        self.ops = {e: [] for e in ENG}
        self.n_dma = n_dma
        self.dma_last = [None] * n_dma
        self.dma_cnt = [0] * n_dma
        self.dma_rr = 0
        self.pending = {}

    def barrier(self):
        lasts = []
        for e in ENG:
            for op in reversed(self.ops[e]):
                if not op.dma:
                    lasts.append(op)
                    break
        lasts += [o for o in self.dma_last if o is not None]
        self.pending = {e: list(lasts) for e in ENG}

    def add(self, eng, fn, reads=(), writes=(), dma=False):
        op = Op(eng, fn, dma)
        deps = op.deps
        if self.pending.get(eng):
            deps.extend(self.pending[eng])
            self.pending[eng] = None
        for b in reads:
            if b.w is not None:
                deps.append(b.w)
        for b in writes:
            w = b.w
            if w is not None and (dma or w.dma or w.eng != eng or eng != "pe"):
                deps.append(w)
            for r in b.r:
                if dma or r.dma or r.eng != eng or eng != "pe":
                    deps.append(r)
        if dma:
            j = self.dma_rr
            self.dma_rr = (j + 1) % self.n_dma
            if self.dma_last[j] is not None:
                deps.append(self.dma_last[j])
            self.dma_cnt[j] += 1
            op.tok = (j, 16 * self.dma_cnt[j])
            self.dma_last[j] = op
        for b in reads:
            if not dma:
                b.r = [r for r in b.r if r.dma or r.eng != eng]
            b.r.append(op)
        for b in writes:
            b.w = op
            b.r = []
        self.ops[eng].append(op)
        return op

    def emit(self, nc, block, eng_sems, dma_sems):
        for e in ENG:
            for op in self.ops[e]:
                for d in op.deps:
                    if not d.dma:
                        d.inc = True
        for e in ENG:
            c = 0
            for op in self.ops[e]:
                if op.dma:
                    op.tok = (dma_sems[op.tok[0]], op.tok[1])
                elif op.inc:
                    c += 1
                    op.tok = (eng_sems[e], c)
        sched = self

        def run(e, h):
            seen = {}
            for op in sched.ops[e]:
                waits = {}
                for d in op.deps:
                    s, v = d.tok
                    k = id(s)
                    if k not in waits or waits[k][1] < v:
                        waits[k] = (s, v)
                for k, (s, v) in waits.items():
                    if seen.get(k, 0) < v:
                        h.wait_ge(s, v)
                        seen[k] = v
                ins = op.fn(h)
                if op.dma:
                    ins.then_inc(op.tok[0], 16)
                elif op.inc:
                    ins.then_inc(op.tok[0], 1)
            if e == "sp":
                for j in range(sched.n_dma):
                    if sched.dma_cnt[j]:
                        h.wait_ge(dma_sems[j], 16 * sched.dma_cnt[j])

        @block.tensor
        def _(h):
            run("pe", h)

        @block.scalar
        def _(h):
            run("act", h)

        @block.vector
        def _(h):
            run("dve", h)

        @block.gpsimd
        def _(h):
            run("pool", h)

        @block.sync
        def _(h):
            run("sp", h)


class Ctx:
    def __init__(self, nc, stack):
        self.nc = nc
        self.S = Sched()
        self.stack = stack
        self.n = 0

    def sb(self, shape, dt=F32, name=None):
        self.n += 1
        t = self.stack.enter_context(self.nc.sbuf_tensor(name or f"sb{self.n}", list(shape), dt))
        return t

    def ps(self, shape, dt=F32, name=None):
        self.n += 1
        t = self.stack.enter_context(self.nc.psum_tensor(name or f"ps{self.n}", list(shape), dt))
        return t

    def dma(self, out, in_, reads=(), writes=(), eng="sp"):
        return self.S.add(eng, lambda h: h.dma_start(out=out, in_=in_), reads, writes, dma=True)

    def mm(self, out, lhsT, rhs, start, stop, reads=(), writes=()):
        return self.S.add("pe", lambda h: h.matmul(out, lhsT, rhs, start=start, stop=stop), reads, writes)

    def tr(self, out, in_, ident, reads=(), writes=()):
        return self.S.add("pe", lambda h: h.matmul(out, in_, ident, start=True, stop=True), reads, writes)

    def act(self, out, in_, func, bias=None, scale=None, reads=(), writes=(), accum_out=None):
        kw = {}
        if bias is not None:
            kw["bias"] = bias
        if scale is not None:
            kw["scale"] = scale
        if accum_out is not None:
            kw["accum_out"] = accum_out
        return self.S.add("act", lambda h: h.activation(out=out, in_=in_, func=func, **kw), reads, writes)

    def ts(self, eng, out, in0, s1, s2, op0, op1=None, reads=(), writes=()):
        if op1 is None:
            return self.S.add(eng, lambda h: h.tensor_scalar(out=out, in0=in0, scalar1=s1, scalar2=None, op0=op0), reads, writes)
        return self.S.add(eng, lambda h: h.tensor_scalar(out=out, in0=in0, scalar1=s1, scalar2=s2, op0=op0, op1=op1), reads, writes)

    def tt(self, eng, out, in0, in1, op, reads=(), writes=()):
        return self.S.add(eng, lambda h: h.tensor_tensor(out=out, in0=in0, in1=in1, op=op), reads, writes)

    def stt(self, eng, out, in0, scalar, in1, op0, op1, reads=(), writes=()):
        return self.S.add(eng, lambda h: h.scalar_tensor_tensor(out=out, in0=in0, scalar=scalar, in1=in1, op0=op0, op1=op1), reads, writes)

    def cp(self, eng, out, in_, reads=(), writes=()):
        if eng == "act":
            return self.S.add("act", lambda h: h.activation(out=out, in_=in_, func=AF.Copy), reads, writes)
        return self.S.add(eng, lambda h: h.tensor_copy(out=out, in_=in_), reads, writes)

    def rsum(self, eng, out, in_, reads=(), writes=()):
        return self.S.add(eng, lambda h: h.reduce_sum(out=out, in_=in_, axis=AX.X), reads, writes)

    def recip(self, out, in_, reads=(), writes=()):
        return self.S.add("dve", lambda h: h.reciprocal(out=out, in_=in_), reads, writes)

    def memset(self, eng, ap, val, writes=()):
        return self.S.add(eng, lambda h: h.memset(ap, val), (), writes)

    def finish(self):
        nc = self.nc
        sems = {}
        for e in ENG:
            sems[e] = self.stack.enter_context(nc.semaphore(f"sem_{e}"))
        dsem = [self.stack.enter_context(nc.semaphore(f"dsem{j}")) for j in range(self.S.n_dma)]
        block = self.stack.enter_context(nc.Block())
        self.S.emit(nc, block, sems, dsem)


class Ring:
    def __init__(self, cx, n, shape, dt=F32, psum=False):
        self.items = []
        for _ in range(n):
            t = cx.ps(shape, dt) if psum else cx.sb(shape, dt)
            self.items.append((t, Buf()))
        self.i = 0

    def next(self):
        it = self.items[self.i]
        self.i = (self.i + 1) % len(self.items)
        return it


NT_F = 2048


def layer_norm_tile(cx, z, zb, out, outb, g_t, b_t, tmp_ring, st_ring):
    st, stb = st_ring.next()
    zc, zcb = tmp_ring.next()
    sq, sqb = tmp_ring.next()
    cx.rsum("dve", st[:, 0:1], z, reads=[zb], writes=[stb])
    cx.ts("dve", st[:, 1:2], st[:, 0:1], -1.0 / D, None, ALU.mult, reads=[stb], writes=[stb])
    cx.ts("dve", zc[:], z, st[:, 1:2], None, ALU.add, reads=[zb, stb], writes=[zcb])
    cx.tt("pool", sq[:], zc[:], zc[:], ALU.mult, reads=[zcb], writes=[sqb])
    cx.rsum("dve", st[:, 2:3], sq[:], reads=[sqb], writes=[stb])
    cx.act(st[:, 3:4], st[:, 2:3], AF.Sqrt, bias=LN_EPS, scale=1.0 / D, reads=[stb], writes=[stb])
    cx.recip(st[:, 4:5], st[:, 3:4], reads=[stb], writes=[stb])
    cx.stt("dve", sq[:], zc[:], st[:, 4:5], g_t[0][:], ALU.mult, ALU.mult, reads=[zcb, stb, g_t[1]], writes=[sqb])
    cx.tt("pool", out, sq[:], b_t[0][:], ALU.add, reads=[sqb, b_t[1]], writes=[outb])


def build_F(n_units=2 * NE, stop=9):
    from contextlib import ExitStack
    nc = bass.Bass("TRN2", target_bir_lowering=False)
    dr = lambda n, s, k="ExternalInput": nc.dram_tensor(n, list(s), F32, kind=k).ap()
    yT = dr("yT", [D, NT_F])
    hin = dr("hin", [NT_F, D])
    w_out = dr("w_out", [D, D])
    ln1g = dr("ln1g", [1, D]); ln1b = dr("ln1b", [1, D])
    ln2g = dr("ln2g", [1, D]); ln2b = dr("ln2b", [1, D])
    router_w = dr("router_w", [D, NE]); router_b = dr("router_b", [1, NE])
    nE_w = max(1, (n_units + 1) // 2)
    w_gu = dr("w_gu", [nE_w, D, 2 * D]); b_gu = dr("b_gu", [NE, 2 * D])
    w_dn = dr("w_dn", [nE_w, D, D]); b_dn = dr("b_dn", [NE, D])
    ident_d = dr("ident", [128, 128])
    out = dr("out", [NT_F, D], "ExternalOutput")

    with ExitStack() as stack:
        cx = Ctx(nc, stack)
        NTILE = NT_F // 128
        ident = cx.sb([128, 128]); identb = Buf()
        h1F = cx.sb([128, 8, NT_F], BF16); h1Fb = [Buf() for _ in range(NTILE)]
        yacc = cx.sb([128, NTILE, D]); yaccb = [Buf() for _ in range(NTILE)]
        G = cx.sb([128, NTILE, NE]); Gb = [Buf() for _ in range(NTILE)]
        bguT = cx.sb([128, 16, NE]); bguTb = Buf()
        bdn = cx.sb([NE, D]); bdnb = Buf()
        cx.dma(ident[:], ident_d[:, :], writes=[identb])
        cx.dma(bdn[:], b_dn[:, :], writes=[bdnb])

        with ExitStack() as sa:
            ca = Ctx(nc, sa); ca.S = cx.S; ca.n = 1000
            wo = ca.sb([128, 8, D], BF16); wob = Buf()
            g1 = (ca.sb([128, D]), Buf()); b1 = (ca.sb([128, D]), Buf())
            rw = ca.sb([128, 8, NE]); rwb = Buf()
            rb = ca.sb([128, NE]); rbb = Buf()
            sa0 = ExitStack(); ca0 = Ctx(nc, sa0); ca0.S = cx.S; ca0.n = 500
            bgu_s = ca0.sb([NE, 2 * D]); bgu_sb = Buf()
            stage = Ring(ca0, 2, [128, 2, D])
            pst = Ring(ca, 2, [128, 512], psum=True)
            ca.dma(g1[0][:], ln1g.partition_broadcast(128), writes=[g1[1]])
            ca.dma(b1[0][:], ln1b.partition_broadcast(128), writes=[b1[1]])
            ca.dma(rb[:], router_b.partition_broadcast(128), writes=[rbb])
            ca.dma(rw[:], router_w.rearrange("(kc p) n -> p kc n", p=128), writes=[rwb])
            ca.dma(bgu_s[:], b_gu[:, :], writes=[bgu_sb])
            wo_v = w_out.rearrange("(kc p) n -> p kc n", p=128)
            for hh in range(4):
                st_t, st_b = stage.next()
                ca.dma(st_t[:], wo_v[:, hh * 2:(hh + 1) * 2, :], writes=[st_b])
                ca.cp("pool", wo[:, hh * 2:(hh + 1) * 2, :], st_t[:], reads=[st_b], writes=[wob])
            for q in range(4):
                pt, pb = pst.next()
                for jj in range(4):
                    j = q * 4 + jj
                    ca.tr(pt[:, jj * NE:(jj + 1) * NE], bgu_s[:, j * 128:(j + 1) * 128], ident[0:NE, 0:NE],
                          reads=[bgu_sb, identb], writes=[pb])
                ca.cp("dve", bguT[:, q * 4:(q + 1) * 4, :], pt[:, 0:4 * NE].rearrange("p (j e) -> p j e", e=NE),
                      reads=[pb], writes=[bguTb])

            sa0.close()
            cx.S.barrier()
            yst = Ring(ca, 2, [128, 8, 256])
            ybf = Ring(ca, 2, [128, 8, 256], BF16)
            hring = Ring(ca, 2, [128, D])
            zring = Ring(ca, 2, [128, D])
            h1ring = Ring(ca, 2, [128, D])
            tmpring = Ring(ca, 2, [128, D])
            stat = Ring(ca, 4, [128, 8])
            h1f32 = Ring(ca, 2, [128, 8, 128])
            psm = Ring(ca, 2, [128, 1024], psum=True)
            psr = Ring(ca, 1, [128, 512], psum=True)
            lg_r = Ring(ca, 2, [128, NE]); ex_r = Ring(ca, 2, [128, NE]); t8_r = Ring(ca, 2, [128, 8])
            gf_r = Ring(ca, 2, [NE, 128])
            yT_v = yT.rearrange("(kc p) t -> p kc t", p=128)
            for tb in range(NT_F // 256 if stop >= 2 else 0):
                ys, ysb = yst.next()
                ca.dma(ys[:], yT_v[:, :, tb * 256:(tb + 1) * 256], writes=[ysb])
                yb, ybb = ybf.next()
                ca.cp("pool", yb[:], ys[:], reads=[ysb], writes=[ybb])
                for ti in range(2):
                    i = tb * 2 + ti
                    ht, hb = hring.next()
                    ca.dma(ht[:], hin[i * 128:(i + 1) * 128, :], writes=[hb])
                    pm, pmb = psm.next()
                    for half in range(2):
                        for kc in range(8):
                            ca.mm(pm[:, half * 512:(half + 1) * 512], yb[:, kc, ti * 128:(ti + 1) * 128],
                                  wo[:, kc, half * 512:(half + 1) * 512], kc == 0, kc == 7,
                                  reads=[ybb, wob], writes=[pmb])
                    z, zb = zring.next()
                    for half in range(2):
                        hs = slice(half * 512, (half + 1) * 512)
                        ca.stt("dve", z[:, hs], ht[:, hs], ALPHA, pm[:, hs], ALU.mult, ALU.add, reads=[hb, pmb], writes=[zb])
                    if stop < 3:
                        continue
                    h1, h1b = h1ring.next()
                    layer_norm_tile(ca, z[:], zb, h1[:], h1b, g1, b1, tmpring, stat)
                    if stop < 3.3:
                        continue
                    hf, hfb = h1f32.next()
                    for q in range(2):
                        pt, pb = pst.next()
                        for jj in range(4):
                            kc = q * 4 + jj
                            ca.tr(pt[:, jj * 128:(jj + 1) * 128], h1[:, kc * 128:(kc + 1) * 128], ident[:],
                                  reads=[h1b, identb], writes=[pb])
                        pv = pt[:].rearrange("p (j t) -> p j t", t=128)
                        ca.cp("dve", hf[:, q * 4:(q + 1) * 4, :], pv, reads=[pb], writes=[hfb])
                        ca.cp("act", h1F[:, q * 4:(q + 1) * 4, i * 128:(i + 1) * 128], hf[:, q * 4:(q + 1) * 4, :],
                              reads=[hfb], writes=[h1Fb[i]])
                    if stop < 5:
                        continue
                    pr, prb = psr.next()
                    for kc in range(8):
                        ca.mm(pr[:, 0:NE], hf[:, kc, :], rw[:, kc, :], kc == 0, kc == 7, reads=[hfb, rwb], writes=[prb])
                    lg, lgb = lg_r.next(); ex, exb = ex_r.next(); t8, t8b = t8_r.next()
                    ca.tt("dve", lg[:], pr[:, 0:NE], rb[:], ALU.add, reads=[prb, rbb], writes=[lgb])
                    ca.S.add("dve", lambda h, o=t8, a=lg: h.max(out=o[:], in_=a[:]), [lgb], [t8b])
                    ca.ts("dve", ex[:], lg[:], t8[:, 3:4], None, ALU.is_ge, reads=[lgb, t8b], writes=[exb])
                    ca.ts("dve", t8[:, 4:5], t8[:, 0:1], -1.0, None, ALU.mult, reads=[t8b], writes=[t8b])
                    ca.act(lg[:], lg[:], AF.Exp, bias=t8[:, 4:5], scale=1.0, reads=[lgb, t8b], writes=[lgb])
                    ca.tt("dve", ex[:], ex[:], lg[:], ALU.mult, reads=[exb, lgb], writes=[exb])
                    ca.rsum("dve", t8[:, 5:6], ex[:], reads=[exb], writes=[t8b])
                    ca.recip(t8[:, 6:7], t8[:, 5:6], reads=[t8b], writes=[t8b])
                    ca.ts("dve", G[:, i, :], ex[:], t8[:, 6:7], None, ALU.mult, reads=[exb, t8b], writes=[Gb[i]])
                    if stop < 6:
                        continue
                    pr2, pr2b = psr.next()
                    ca.tr(pr2[0:NE, 0:128], G[:, i, :], ident[:], reads=[Gb[i], identb], writes=[pr2b])
                    gf, gfb = gf_r.next()
                    ca.cp("dve", gf[:], pr2[0:NE, 0:128], reads=[pr2b], writes=[gfb])
                    pm2, pm2b = psm.next()
                    for half in range(2):
                        ca.mm(pm2[:, half * 512:(half + 1) * 512], gf[:], bdn[:, half * 512:(half + 1) * 512], True, True,
                              reads=[gfb, bdnb], writes=[pm2b])
                    for half in range(2):
                        hs = slice(half * 512, (half + 1) * 512)
                        ca.stt("dve", yacc[:, i, hs], h1[:, hs], ALPHA, pm2[:, hs], ALU.mult, ALU.add,
                               reads=[h1b, pm2b], writes=[yaccb[i]])

        cx.S.barrier()
        with ExitStack() as sb_:
            cb = Ctx(nc, sb_); cb.S = cx.S; cb.n = 2000
            stage = Ring(cb, 3, [128, 2048])
            wg_r = Ring(cb, 2, [128, 8, 512], BF16)
            wl_r = Ring(cb, 2, [128, 8, 512], BF16)
            wd_r = Ring(cb, 2, [128, 4, D], BF16)
            actr = Ring(cb, 2, [128, 4, 512], BF16)
            glu_r = Ring(cb, 2, [128, 512]); sig_r = Ring(cb, 2, [128, 512]); lin_r = Ring(cb, 2, [128, 512])
            psg = Ring(cb, 2, [128, 512], psum=True)
            psl = Ring(cb, 2, [128, 512], psum=True)
            pso = Ring(cb, 2, [128, 512], psum=True)
            for u in range(n_units):
                e, hf_ = u // 2, u % 2
                wg, wgb = wg_r.next(); wl, wlb = wl_r.next(); wd, wdb = wd_r.next()
                gu_v = w_gu[e].rearrange("(kc p) n -> p kc n", p=128)
                dn_v = w_dn[e].rearrange("(j p) n -> p j n", p=128)
                for (dst, dstb, c0) in ((wg, wgb, hf_ * 512), (wl, wlb, D + hf_ * 512)):
                    for q in range(2):
                        st_t, st_b = stage.next()
                        cb.dma(st_t[:].rearrange("p (k n) -> p k n", n=512), gu_v[:, q * 4:(q + 1) * 4, c0:c0 + 512], writes=[st_b])
                        cb.cp("pool", dst[:, q * 4:(q + 1) * 4, :], st_t[:].rearrange("p (k n) -> p k n", n=512),
                              reads=[st_b], writes=[dstb])
                for q in range(2):
                    st_t, st_b = stage.next()
                    cb.dma(st_t[:].rearrange("p (k n) -> p k n", n=D), dn_v[:, hf_ * 4 + q * 2: hf_ * 4 + q * 2 + 2, :], writes=[st_b])
                    cb.cp("pool", wd[:, q * 2:(q + 1) * 2, :], st_t[:].rearrange("p (k n) -> p k n", n=D),
                          reads=[st_b], writes=[wdb])
                for tb in range(NT_F // 512):
                    tsl = slice(tb * 512, (tb + 1) * 512)
                    rd_h = [h1Fb[tb * 4 + q] for q in range(4)]
                    ac, acb = actr.next()
                    for j in range(4):
                        pg, pgb = psg.next(); pl, plb = psl.next()
                        for kc in range(8):
                            cb.mm(pg[:], wg[:, kc, j * 128:(j + 1) * 128], h1F[:, kc, tsl], kc == 0, kc == 7,
                                  reads=[wgb] + rd_h, writes=[pgb])
                        for kc in range(8):
                            cb.mm(pl[:], wl[:, kc, j * 128:(j + 1) * 128], h1F[:, kc, tsl], kc == 0, kc == 7,
                                  reads=[wlb] + rd_h, writes=[plb])
                        jg = hf_ * 4 + j
                        glu, glub = glu_r.next(); sig, sigb = sig_r.next(); lin, linb = lin_r.next()
                        cb.ts("dve", glu[:], pg[:], bguT[:, jg, e:e + 1], 7.0, ALU.add, ALU.min, reads=[pgb, bguTb], writes=[glub])
                        cb.act(sig[:], glu[:], AF.Sigmoid, scale=1.702, reads=[glub], writes=[sigb])
                        cb.ts("dve", lin[:], pl[:], bguT[:, 8 + jg, e:e + 1], 7.0, ALU.add, ALU.min, reads=[plb, bguTb], writes=[linb])
                        cb.ts("pool", lin[:], lin[:], -7.0, 1.0, ALU.max, ALU.add, reads=[linb], writes=[linb])
                        cb.tt("pool", glu[:], glu[:], sig[:], ALU.mult, reads=[glub, sigb], writes=[glub])
                        cb.tt("dve", ac[:, j, :], glu[:], lin[:], ALU.mult, reads=[glub, linb], writes=[acb])
                    for ti in range(4):
                        i = tb * 4 + ti
                        for half in range(2):
                            po, pob = pso.next()
                            for j in range(4):
                                cb.mm(po[:], ac[:, j, ti * 128:(ti + 1) * 128], wd[:, j, half * 512:(half + 1) * 512],
                                      j == 0, j == 3, reads=[acb, wdb], writes=[pob])
                            ysl = yacc[:, i, half * 512:(half + 1) * 512]
                            cb.stt("dve", ysl, po[:], G[:, i, e:e + 1], ysl, ALU.mult, ALU.add,
                                   reads=[pob, Gb[i], yaccb[i]], writes=[yaccb[i]])

        cx.S.barrier()
        with ExitStack() as sc:
            cc = Ctx(nc, sc); cc.S = cx.S; cc.n = 3000
            g2 = (cc.sb([128, D]), Buf()); b2 = (cc.sb([128, D]), Buf())
            cc.dma(g2[0][:], ln2g.partition_broadcast(128), writes=[g2[1]])
            cc.dma(b2[0][:], ln2b.partition_broadcast(128), writes=[b2[1]])
            tmpring = Ring(cc, 2, [128, D]); stat = Ring(cc, 4, [128, 8]); oring = Ring(cc, 3, [128, D])
            outb = Buf()
            for i in range(NTILE):
                o, ob = oring.next()
                layer_norm_tile(cc, yacc[:, i, :], yaccb[i], o[:], ob, g2, b2, tmpring, stat)
                cc.dma(out[i * 128:(i + 1) * 128, :], o[:], reads=[ob], writes=[outb])
        cx.finish()
    return nc


SEQ = 8192
TB = 512
HALO = 16
LCH = 64
C0 = float(np.exp(-0.5))
GN_EPS = 64e-5
RW_COLS = 448
ML_QK = 128
ML_VOG = 130


def bc3(ap2, n):
    return ap2.unsqueeze(2).to_broadcast([ap2.shape[0], ap2.shape[1], n])


def build_M(layer2=False, nblk=SEQ // TB):
    from contextlib import ExitStack
    nc = bass.Bass("TRN2", target_bir_lowering=False)
    dr = lambda n, s, k="ExternalInput": nc.dram_tensor(n, list(s), F32, kind=k).ap()
    hT = dr("hT", [D, SEQ])
    consts = dr("consts", [128, 1024])
    rw_w = [dr(f"rw_w{s}", [D, RW_COLS]) for s in range(2)]
    rw_tab = [dr(f"rw_tab{s}", [128, 16]) for s in range(2)]
    rw_w2 = [dr(f"rw_w2{s}", [64, 64]) for s in range(2)]
    rw_a2 = [dr(f"rw_a2{s}", [64, 64]) for s in range(2)]
    rw_g2 = [dr(f"rw_g2{s}", [128, 64]) for s in range(2)]
    rw_rows = [dr(f"rw_rows{s}", [3, 64]) for s in range(2)]
    if layer2:
        rw_v1 = dr("rw_v1", [D, 32])
        rw_v2 = [dr(f"rw_v2{s}", [32, 64]) for s in range(2)]
        rw_vf = [dr(f"rw_vf{s}", [64, SEQ]) for s in range(2)]
    ml_wqk = [dr(f"ml_wqk{s}", [D, ML_QK]) for s in range(2)]
    ml_wvog = [dr(f"ml_wvog{s}", [D, ML_VOG]) for s in range(2)]
    ml_tab = [dr(f"ml_tab{s}", [64, 16]) for s in range(2)]
    ml_rows = [dr(f"ml_rows{s}", [3, 64]) for s in range(2)]
    pl_w = dr("pl_w", [D, 64])
    pl_pw = dr("pl_pw", [64, 64])
    pl_tab = dr("pl_tab", [64, 40])
    y_rw = [dr(f"y_rw{s}", [SEQ, 64], "ExternalOutput") for s in range(2)]
    y_ml = [dr(f"y_ml{s}", [SEQ, 64], "ExternalOutput") for s in range(2)]
    y_pl = dr("y_pl", [64, SEQ], "ExternalOutput")
    if not layer2:
        vf_out = [dr(f"vf_out{s}", [64, SEQ], "ExternalOutput") for s in range(2)]

    with ExitStack() as stack:
        cx = Ctx(nc, stack)
        cst = cx.sb([128, 1024]); cstb = Buf()
        cx.dma(cst[:], consts[:, :], writes=[cstb])
        ident = cst[:, 0:128]
        tri128 = cst[:, 128:256]
        mu128 = cst[:, 256:384]
        mm64 = cst[0:64, 384:512]
        ml64 = cst[0:64, 512:576]
        ones = cst[:, 576:640]
        triI = cst[:, 640:768]
        triE = cst[:, 768:896]
        identb = cx.sb([128, 128], BF16); identbb = Buf()
        cx.cp("dve", identb[:], ident, reads=[cstb], writes=[identbb])
        mm64x4 = cx.sb([64, 4, 128]); ml64x8 = cx.sb([64, 8, 64]); i64x8 = cx.sb([64, 8, 64]); repb = Buf()
        for c in range(4):
            cx.cp("dve", mm64x4[:, c, :], mm64, reads=[cstb], writes=[repb])
        for c in range(8):
            cx.cp("dve", ml64x8[:, c, :], ml64, reads=[cstb], writes=[repb])
            cx.cp("pool", i64x8[:, c, :], cst[0:64, 0:64], reads=[cstb], writes=[repb])

        wfill = []

        def load_w(dram, ncols):
            t = cx.sb([128, 8, ncols], BF16); b = Buf()
            wfill.append((t, b, dram, ncols))
            return t, b
        rwW = [load_w(rw_w[s], RW_COLS) for s in range(2)]
        mlWqk = [load_w(ml_wqk[s], ML_QK) for s in range(2)]
        mlWvog = [load_w(ml_wvog[s], ML_VOG) for s in range(2)]
        plW = load_w(pl_w, 64)
        if layer2:
            v1W = load_w(rw_v1, 32)

        def small(dram, shape, dt=F32, bcast=False):
            t = cx.sb(shape); b = Buf()
            cx.dma(t[:], dram.partition_broadcast(shape[0]) if bcast else dram, writes=[b])
            if dt == F32:
                return t, b
            t2 = cx.sb(shape, dt); b2 = Buf()
            cx.cp("dve", t2[:], t[:], reads=[b], writes=[b2])
            return t2, b2

        rwtab = []; rww2 = []; rwa2 = []; rwg2 = []; rwrow = []; rwv2 = []
        for s in range(2):
            t, b = small(rw_tab[s][:, :], [128, 16])
            om = cx.sb([128, 8]); omb = Buf()
            cx.ts("dve", om[:, 0:6], t[:, 0:6], -1.0, 1.0, ALU.mult, ALU.add, reads=[b], writes=[omb])
            cx.ts("dve", om[:, 6:7], t[:, 9:10], -1.0, 1.0, ALU.mult, ALU.add, reads=[b], writes=[omb])
            rwtab.append((t, b, om, omb))
            rww2.append(small(rw_w2[s][:, :], [64, 64], BF16))
            rwa2.append(small(rw_a2[s][:, :], [64, 64], BF16))
            rwg2.append(small(rw_g2[s][:, :], [128, 64], BF16))
            w0r = small(rw_rows[s][0:1, :], [128, 64], bcast=True)
            gr = small(rw_rows[s][1:2, :], [64, 64], bcast=True)
            br = small(rw_rows[s][2:3, :], [64, 64], bcast=True)
            gb = cx.sb([64, 2, 8, 64]); gbb = Buf()
            for c in range(8):
                cx.cp("pool", gb[:, 0, c, :], gr[0][:], reads=[gr[1]], writes=[gbb])
                cx.cp("pool", gb[:, 1, c, :], br[0][:], reads=[br[1]], writes=[gbb])
            rwrow.append((w0r, gb, gbb))
            if layer2:
                rwv2.append(small(rw_v2[s][:, :], [32, 64], BF16))
        mltab = []; mlrow = []
        for s in range(2):
            mltab.append(small(ml_tab[s][:, :], [64, 16]))
            rows = [small(ml_rows[s][j:j + 1, :], [128, 64], bcast=True) for j in range(3)]
            ng = cx.sb([128, 4, 64]); ngb = Buf()
            for c in range(4):
                cx.cp("pool", ng[:, c, :], rows[0][0][:], reads=[rows[0][1]], writes=[ngb])
            nbf = cx.sb([128, 1]); nbfb = Buf()
            cx.ts("dve", nbf[:], rows[2][0][:, 0:1], -1.0, None, ALU.mult, reads=[rows[2][1]], writes=[nbfb])
            mlrow.append((ng, ngb, rows[1], nbf, nbfb))
        pltab = small(pl_tab[:, :], [64, 40])
        plpw = small(pl_pw[:, :], [64, 64])

        def zeros(shape, dt=F32):
            t = cx.sb(shape, dt); b = Buf()
            cx.memset("pool", t[:], 0.0, writes=[b])
            return t, b
        NRW = 6
        rwHalo = [[zeros([128, HALO]) for g in range(NRW)] for s in range(2)]
        mlHalo = [[zeros([64, HALO]) for g in range(2)] for s in range(2)]
        plHalo = zeros([64, HALO])
        rwH = [[zeros([64, 64]), zeros([64, 64])] for s in range(2)]
        mlC = [[zeros([64, 65], BF16), zeros([64, 65], BF16)] for s in range(2)]
        hidx = [0, 0]; cidx = [0, 0]

        s0 = ExitStack(); c0x = Ctx(nc, s0); c0x.S = cx.S; c0x.n = 5000
        wst = Ring(c0x, 2, [128, 8, 448])
        for (t, b, dram, ncols) in wfill:
            st_t, st_b = wst.next()
            cx.dma(st_t[:, :, 0:ncols], dram.rearrange("(kc p) n -> p kc n", p=128), writes=[st_b])
            cx.cp("pool", t[:], st_t[:, :, 0:ncols], reads=[st_b], writes=[b])
        s0.close()
        cx.S.barrier()

        tiles = {}

        def T(name, cols=TB, dt=F32, parts=128):
            if name not in tiles:
                tiles[name] = (cx.sb([parts, cols], dt), Buf())
            return tiles[name]

        hst = Ring(cx, 2, [128, 8, 256])
        hbr = Ring(cx, 2, [128, 8, TB], BF16)
        psA = Ring(cx, 2, [128, 512], psum=True)
        psB = Ring(cx, 1, [128, 1024], psum=True)
        psC = Ring(cx, 4, [128, 512], psum=True)
        pwr = Ring(cx, 2, [128, HALO + TB])
        tmpr = Ring(cx, 2, [128, TB])
        sm = Ring(cx, 6, [128, 32])
        oring = Ring(cx, 2, [128, TB])
        hT_v = hT.rearrange("(kc p) t -> p kc t", p=128)

        def proj_halo(W, Wb, c0_, n, hb, hbb, halo):
            p, pb = psA.next()
            for kc in range(8):
                cx.mm(p[0:n, :], W[:, kc, c0_:c0_ + n], hb[:, kc, :], kc == 0, kc == 7, reads=[Wb, hbb], writes=[pb])
            P_, Pb = pwr.next()
            cx.cp("act", P_[0:n, HALO:], p[0:n, :], reads=[pb], writes=[Pb])
            cx.cp("pool", P_[0:n, 0:HALO], halo[0][0:n, :], reads=[halo[1]], writes=[Pb])
            cx.cp("pool", halo[0][0:n, :], P_[0:n, TB:TB + HALO], reads=[Pb], writes=[halo[1]])
            return P_, Pb

        def groupnorm(src3, srcb, P, nch, eps, dst, dstb):
            st, stb = sm.next()
            sq, sqb = T("gn_sq")
            sq3 = sq[0:P, 0:nch * 64].rearrange("p (c v) -> p c v", v=64)
            cx.rsum("dve", st[0:P, 0:nch], src3, reads=[srcb], writes=[stb])
            cx.ts("dve", st[0:P, 8:8 + nch], st[0:P, 0:nch], -1.0 / 64, None, ALU.mult, reads=[stb], writes=[stb])
            cx.tt("dve", dst, src3, bc3(st[0:P, 8:8 + nch], 64), ALU.add, reads=[srcb, stb], writes=[dstb])
            cx.tt("pool", sq3, dst, dst, ALU.mult, reads=[dstb], writes=[sqb])
            cx.rsum("dve", st[0:P, 16:16 + nch], sq3, reads=[sqb], writes=[stb])
            cx.act(st[0:P, 24:24 + nch], st[0:P, 16:16 + nch], AF.Sqrt, bias=eps, scale=1.0 / 64, reads=[stb], writes=[stb])
            cx.recip(st[0:P, 0:nch], st[0:P, 24:24 + nch], reads=[stb], writes=[stb])
            cx.tt("dve", dst, dst, bc3(st[0:P, 0:nch], 64), ALU.mult, reads=[dstb, stb], writes=[dstb])

        def v3(t, x, n=None, parts=64):
            ap = t[0:parts, :] if n is None else t[0:parts, 0:n]
            return ap.rearrange("p (c x) -> p c x", x=x)

        for blk in range(nblk):
            t0 = blk * TB
            hb, hbb = hbr.next()
            for hh in range(2):
                hs, hsb = hst.next()
                cx.dma(hs[:], hT_v[:, :, t0 + hh * 256:t0 + (hh + 1) * 256], writes=[hsb])
                cx.cp("pool", hb[:, :, hh * 256:(hh + 1) * 256], hs[:], reads=[hsb], writes=[hbb])

            P_, Pb = proj_halo(plW[0], plW[1], 0, 64, hb, hbb, plHalo)
            acc, accb = T("pl_acc")
            tb_, tbb = pltab
            cx.ts("dve", acc[0:64, :], P_[0:64, HALO:], tb_[:, 0:1], None, ALU.mult, reads=[Pb, tbb], writes=[accb])
            for i in range(1, 16):
                cx.stt("dve", acc[0:64, :], P_[0:64, HALO - i:HALO - i + TB], tb_[:, i:i + 1], acc[0:64, :],
                       ALU.mult, ALU.add, reads=[Pb, tbb, accb], writes=[accb])
            dd, ddb = T("pl_dd")
            cx.stt("dve", dd[0:64, :], acc[0:64, :], tb_[:, 16:17], P_[0:64, HALO:], ALU.mult, ALU.subtract,
                   reads=[accb, tbb, Pb], writes=[ddb])
            if blk == 0:
                t16, t16b = sm.next()
                cx.tt("dve", t16[0:64, 0:16], acc[0:64, 0:16], tb_[:, 20:36], ALU.mult, reads=[accb, tbb], writes=[t16b])
                cx.tt("dve", dd[0:64, 0:16], t16[0:64, 0:16], P_[0:64, HALO:HALO + 16], ALU.subtract, reads=[t16b, Pb], writes=[ddb])
            p2, p2b = psA.next()
            cx.mm(p2[0:64, :], plpw[0][:], dd[0:64, :], True, True, reads=[plpw[1], ddb], writes=[p2b])
            o, ob = oring.next()
            cx.ts("dve", o[0:64, :], p2[0:64, :], tb_[:, 17:18], None, ALU.mult, reads=[p2b, tbb], writes=[ob])
            cx.dma(y_pl[:, t0:t0 + TB], o[0:64, :], reads=[ob], writes=[Buf()])

            for s in range(2):
                mt, mtb = mltab[s]
                ng, ngb, (bi_t, bi_b), nbf, nbfb = mlrow[s]
                qk = []
                for g in range(2):
                    P_, Pb = proj_halo(mlWqk[s][0], mlWqk[s][1], g * 64, 64, hb, hbb, mlHalo[s][g])
                    acc, accb = T("ml_acc")
                    cx.ts("dve", acc[0:64, :], P_[0:64, HALO - 3:HALO - 3 + TB], mt[:, g * 5:g * 5 + 1], None, ALU.mult,
                          reads=[Pb, mtb], writes=[accb])
                    for i in range(1, 4):
                        cx.stt("dve", acc[0:64, :], P_[0:64, HALO - 3 + i:HALO - 3 + i + TB], mt[:, g * 5 + i:g * 5 + i + 1],
                               acc[0:64, :], ALU.mult, ALU.add, reads=[Pb, mtb, accb], writes=[accb])
                    cx.act(acc[0:64, :], acc[0:64, :], AF.Silu, bias=mt[:, g * 5 + 4:g * 5 + 5], scale=1.0, reads=[accb, mtb], writes=[accb])
                    qb_, qbb = T("ml_qF" if g == 0 else "ml_kF", TB, BF16)
                    if g == 0:
                        cx.ts("dve", qb_[0:64, :], acc[0:64, :], 0.125, None, ALU.mult, reads=[accb], writes=[qbb])
                    else:
                        cx.cp("dve", qb_[0:64, :], acc[0:64, :], reads=[accb], writes=[qbb])
                    qk.append((qb_, qbb))
                (qF, qFb), (kF, kFb) = qk
                vog2, vog2b = T("ml_vo")
                gts, gtsb = sm.next()
                for half in range(2):
                    pv, pvb = psA.next()
                    for tt_ in range(2):
                        ti = half * 2 + tt_
                        for kc in range(8):
                            cx.mm(pv[:, tt_ * ML_VOG:(tt_ + 1) * ML_VOG], hb[:, kc, ti * 128:(ti + 1) * 128],
                                  mlWvog[s][0][:, kc, :], kc == 0, kc == 7, reads=[hbb, mlWvog[s][1]], writes=[pvb])
                    pv3 = pv[:, 0:2 * ML_VOG].rearrange("p (t c) -> p t c", c=ML_VOG)
                    cx.cp("dve", vog2[:, half * 256:(half + 1) * 256].rearrange("p (t c) -> p t c", c=128), pv3[:, :, 0:128],
                          reads=[pvb], writes=[vog2b])
                    cx.cp("dve", gts[:, half * 4:half * 4 + 4].rearrange("p (t c) -> p t c", c=2), pv3[:, :, 128:130],
                          reads=[pvb], writes=[gtsb])
                vo3 = vog2[:, :].rearrange("p (t c) -> p t c", c=128)
                g3 = gts[:, 0:8].rearrange("p (t c) -> p t c", c=2)
                cx.act(gts[:, 8:12], g3[:, :, 1], AF.Exp, bias=nbf[:], scale=-1.0, reads=[gtsb, nbfb], writes=[gtsb])
                cx.act(gts[:, 12:16], gts[:, 8:12], AF.Ln, bias=1.0, scale=1.0, reads=[gtsb], writes=[gtsb])
                pg, pgb = psC.next()
                cx.mm(pg[:, 0:4], tri128, gts[:, 12:16], True, True, reads=[cstb, gtsb], writes=[pgb])
                cx.mm(pg[0:64, 8:12], ones[:, 0:64], gts[:, 12:16], True, True, reads=[cstb, gtsb], writes=[pgb])
                cx.tt("dve", gts[:, 16:20], g3[:, :, 0], pg[:, 0:4], ALU.add, reads=[gtsb, pgb], writes=[gtsb])
                cx.act(gts[:, 20:24], gts[:, 16:20], AF.Exp, bias=bi_t[:, 0:1], scale=1.0, reads=[gtsb, bi_b], writes=[gtsb])
                egl, eglb = sm.next()
                cx.cp("dve", egl[:, 8:12], pg[:, 0:4], reads=[pgb], writes=[eglb])
                cx.cp("dve", egl[0:64, 12:16], pg[0:64, 8:12], reads=[pgb], writes=[eglb])
                cx.act(gts[:, 24:28], egl[:, 8:12], AF.Exp, scale=-1.0, reads=[eglb], writes=[gtsb])
                cx.act(egl[0:64, 0:4], egl[0:64, 12:16], AF.Exp, scale=-1.0, reads=[eglb], writes=[eglb])
                pk, pkb = psC.next()
                for ti in range(4):
                    cx.mm(pk[:, ti * 64:(ti + 1) * 64], kF[0:64, ti * 128:(ti + 1) * 128], identb[0:64, 0:64], True, True,
                          reads=[kFb, identbb], writes=[pkb])
                kT, kTb = T("ml_kT", TB, BF16)
                cx.cp("dve", kT[:, 0:256], pk[:, 0:256], reads=[pkb], writes=[kTb])
                ps_, psb = psC.next()
                for ti in range(4):
                    cx.mm(ps_[:, ti * 128:(ti + 1) * 128], kF[0:64, ti * 128:(ti + 1) * 128], qF[0:64, ti * 128:(ti + 1) * 128],
                          True, True, reads=[kFb, qFb], writes=[psb])
                wp, wpb = T("ml_wp", TB, BF16)
                va, vab = T("ml_va", TB, BF16)
                av, avb = T("ml_av", TB, BF16)
                va3 = va[:, 0:260].rearrange("p (t c) -> p t c", c=65)
                av3 = av[:, 0:260].rearrange("p (t c) -> p t c", c=65)
                cx.memset("pool", va[:, 0:260], 1.0, writes=[vab])
                cx.cp("pool", va3[:, :, 0:64], vo3[:, :, 0:64], reads=[vog2b], writes=[vab])
                for ti in range(4):
                    cx.stt("dve", wp[:, ti * 128:(ti + 1) * 128], ps_[:, ti * 128:(ti + 1) * 128], gts[:, 20 + ti:21 + ti], mu128,
                           ALU.mult, ALU.mult, reads=[psb, gtsb, cstb], writes=[wpb])
                    cx.ts("dve", av3[:, ti, :], va3[:, ti, :], gts[:, 20 + ti:21 + ti], None, ALU.mult, reads=[vab, gtsb], writes=[avb])
                pn, pnb = psA.next()
                for ti in range(4):
                    Cc, Ccb = mlC[s][cidx[s]]
                    Cn, Cnb = mlC[s][1 - cidx[s]]
                    cidx[s] = 1 - cidx[s]
                    cx.mm(pn[:, ti * 65:(ti + 1) * 65], wp[:, ti * 128:(ti + 1) * 128], va3[:, ti, :], True, False,
                          reads=[wpb, vab], writes=[pnb])
                    cx.mm(pn[:, ti * 65:(ti + 1) * 65], qF[0:64, ti * 128:(ti + 1) * 128], Cc[:], False, True,
                          reads=[qFb, Ccb], writes=[pnb])
                    pc, pcb = psC.next()
                    cx.mm(pc[0:64, 0:65], kT[:, ti * 64:(ti + 1) * 64], av3[:, ti, :], True, False, reads=[kTb, avb], writes=[pcb])
                    cx.mm(pc[0:64, 0:65], identb[0:64, 0:64], Cc[:], False, True, reads=[identbb, Ccb], writes=[pcb])
                    cx.ts("dve", Cn[:], pc[0:64, 0:65], egl[0:64, ti:ti + 1], None, ALU.mult, reads=[pcb, eglb], writes=[Cnb])
                nm, nmb = T("ml_nm")
                cx.cp("dve", nm[:, 0:260], pn[:, 0:260], reads=[pnb], writes=[nmb])
                pn3 = nm[:, 0:260].rearrange("p (t c) -> p t c", c=65)
                cx.tt("dve", gts[:, 28:32], pn3[:, :, 64], gts[:, 24:28], ALU.mult, reads=[nmb, gtsb], writes=[gtsb])
                ab, abb = sm.next()
                cx.ts("dve", ab[:, 0:4], gts[:, 28:32], -1.0, None, ALU.mult, reads=[gtsb], writes=[abb])
                cx.tt("dve", ab[:, 0:4], ab[:, 0:4], gts[:, 28:32], ALU.max, reads=[abb, gtsb], writes=[abb])
                cx.ts("dve", gts[:, 28:32], ab[:, 0:4], 1.0, None, ALU.max, reads=[abb], writes=[gtsb])
                cx.recip(gts[:, 28:32], gts[:, 28:32], reads=[gtsb], writes=[gtsb])
                cx.tt("dve", gts[:, 28:32], gts[:, 28:32], gts[:, 24:28], ALU.mult, reads=[gtsb], writes=[gtsb])
                hr, hrb = T("ml_hr")
                hr3 = hr[:, 0:256].rearrange("p (t c) -> p t c", c=64)
                cx.tt("dve", hr3, pn3[:, :, 0:64], bc3(gts[:, 28:32], 64), ALU.mult, reads=[nmb, gtsb], writes=[hrb])
                hn, hnb = T("ml_hn")
                hn3 = hn[:, 0:256].rearrange("p (t c) -> p t c", c=64)
                groupnorm(hr3, hrb, 128, 4, LN_EPS, hn3, hnb)
                so, sob = T("ml_so")
                so3 = so[:, 0:256].rearrange("p (t c) -> p t c", c=64)
                cx.act(so3, vo3[:, :, 64:128], AF.Sigmoid, reads=[vog2b], writes=[sob])
                cx.tt("pool", hn3, hn3, ng[:], ALU.mult, reads=[hnb, ngb], writes=[hnb])
                o, ob = oring.next()
                o3 = o[:, 0:256].rearrange("p (t c) -> p t c", c=64)
                cx.tt("dve", o3, hn3, so3, ALU.mult, reads=[hnb, sob], writes=[ob])
                cx.dma(y_ml[s][t0:t0 + TB, :].rearrange("(c t) v -> t c v", t=128), o3, reads=[ob], writes=[Buf()])

            for s in range(2):
                tab, tabb, om, omb = rwtab[s]
                W, Wb = rwW[s]
                X = []
                for g in range(NRW):
                    n = 128 if g == 5 else 64
                    P_, Pb = proj_halo(W, Wb, g * 64, n, hb, hbb, rwHalo[s][g])
                    tmp, tmpb = tmpr.next()
                    x, xb = T(f"rw_x{g}")
                    cx.ts("dve", tmp[0:n, :], P_[0:n, HALO - 1:HALO - 1 + TB], tab[0:n, g:g + 1], None, ALU.mult, reads=[Pb, tabb], writes=[tmpb])
                    cx.stt("dve", x[0:n, :], P_[0:n, HALO:], om[0:n, g:g + 1], tmp[0:n, :], ALU.mult, ALU.add,
                           reads=[Pb, omb, tmpb], writes=[xb])
                    X.append((x, xb))
                (r, rb_), (k0, k0b), (v, vb), (wd, wdb), (ad, adb), (gd, gdb) = X
                if layer2:
                    p, pb = psA.next()
                    for kc in range(8):
                        cx.mm(p[0:32, :], v1W[0][:, kc, :], hb[:, kc, :], kc == 0, kc == 7, reads=[v1W[1], hbb], writes=[pb])
                    hv, hvb = T("rw_adh", TB, BF16)
                    cx.cp("act", hv[0:32, :], p[0:32, :], reads=[pb], writes=[hvb])
                    p2, p2b = psA.next()
                    cx.mm(p2[0:64, :], rwv2[s][0][:], hv[0:32, :], True, True, reads=[rwv2[s][1], hvb], writes=[p2b])
                    vg, vgb = T("rw_sq")
                    cx.act(vg[0:64, :], p2[0:64, :], AF.Sigmoid, bias=tab[0:64, 11:12], scale=1.0, reads=[p2b, tabb], writes=[vgb])
                    vf, vfb = T("rw_rkr")
                    cx.dma(vf[0:64, :], rw_vf[s][:, t0:t0 + TB], writes=[vfb])
                    cx.tt("pool", vf[0:64, :], vf[0:64, :], v[0:64, :], ALU.subtract, reads=[vfb, vb], writes=[vfb])
                    cx.tt("pool", vf[0:64, :], vf[0:64, :], vg[0:64, :], ALU.mult, reads=[vfb, vgb], writes=[vfb])
                    cx.tt("dve", v[0:64, :], v[0:64, :], vf[0:64, :], ALU.add, reads=[vb, vfb], writes=[vb])
                else:
                    cx.dma(vf_out[s][:, t0:t0 + TB], v[0:64, :], reads=[vb], writes=[Buf()])
                twd, twdb = T("rw_twd", TB, BF16); adh, adhb = T("rw_adh", TB, BF16); sgd, sgdb = T("rw_sgd", TB, BF16)
                cx.act(twd[0:64, :], wd[0:64, :], AF.Tanh, reads=[wdb], writes=[twdb])
                cx.cp("pool", adh[0:64, :], ad[0:64, :], reads=[adb], writes=[adhb])
                cx.act(sgd[:, :], gd[:, :], AF.Sigmoid, reads=[gdb], writes=[sgdb])
                (w0row, w0rb), gbt, gbtb = rwrow[s]
                sg, sgb = T("rw_sg")
                pd, pdb = psC.next()
                for ti in range(4):
                    cx.mm(pd[:, ti * 64:(ti + 1) * 64], twd[0:64, ti * 128:(ti + 1) * 128], rww2[s][0][:], True, True,
                          reads=[twdb, rww2[s][1]], writes=[pdb])
                for ti in range(4):
                    cx.tt("dve", sg[:, ti * 64:(ti + 1) * 64], pd[:, ti * 64:(ti + 1) * 64], w0row[:], ALU.add,
                          reads=[pdb, w0rb], writes=[sgb])
                cx.act(sg[:, 0:256], sg[:, 0:256], AF.Sigmoid, reads=[sgb], writes=[sgb])
                gi, gib = T("rw_gi"); ge, geb = T("rw_ge"); gv, gvb = T("rw_gv")
                pci, pcib = psC.next()
                for ti in range(4):
                    cx.mm(pci[0:64, ti * 128:(ti + 1) * 128], sg[:, ti * 64:(ti + 1) * 64], triI, True, True, reads=[sgb, cstb], writes=[pcib])
                cx.act(gi[0:64, :], pci[0:64, :], AF.Exp, scale=-C0, reads=[pcib], writes=[gib])
                cx.act(gv[0:64, :], gi[0:64, :], AF.Copy, reads=[gib], writes=[gvb])
                cx.recip(gv[0:64, :], gv[0:64, :], reads=[gvb], writes=[gvb])
                pce, pceb = psC.next()
                for ti in range(4):
                    cx.mm(pce[0:64, ti * 128:(ti + 1) * 128], sg[:, ti * 64:(ti + 1) * 64], triE, True, True, reads=[sgb, cstb], writes=[pceb])
                cx.act(ge[0:64, :], pce[0:64, :], AF.Exp, scale=-C0, reads=[pceb], writes=[geb])
                gL, gLb = sm.next()
                cx.cp("dve", gL[0:64, 0:8], gi[0:64, :].rearrange("p (c t) -> p c t", t=64)[:, :, 63], reads=[gib], writes=[gLb])
                pa, pab = psA.next()
                cx.mm(pa[0:64, :], rwa2[s][0][:], adh[0:64, :], True, True, reads=[rwa2[s][1], adhb], writes=[pab])
                asg, asgb = T("rw_asg")
                cx.act(asg[0:64, :], pa[0:64, :], AF.Sigmoid, bias=tab[0:64, 7:8], scale=1.0, reads=[pab, tabb], writes=[asgb])
                kks, kksb = T("rw_kk"); sq, sqb = T("rw_sq")
                cx.ts("dve", kks[0:64, :], k0[0:64, :], tab[0:64, 8:9], None, ALU.mult, reads=[k0b, tabb], writes=[kksb])
                cx.tt("pool", sq[0:64, :], kks[0:64, :], kks[0:64, :], ALU.mult, reads=[kksb], writes=[sqb])
                pss, pssb = psA.next()
                cx.mm(pss[0:64, :], ones[0:64, 0:64], sq[0:64, :], True, True, reads=[cstb, sqb], writes=[pssb])
                cx.act(sq[0:64, :], pss[0:64, :], AF.Sqrt, reads=[pssb], writes=[sqb])
                cx.ts("dve", sq[0:64, :], sq[0:64, :], 1e-12, None, ALU.max, reads=[sqb], writes=[sqb])
                cx.recip(sq[0:64, :], sq[0:64, :], reads=[sqb], writes=[sqb])
                cx.tt("dve", kks[0:64, :], kks[0:64, :], sq[0:64, :], ALU.mult, reads=[kksb, sqb], writes=[kksb])
                km, kmb = T("rw_km")
                cx.ts("dve", km[0:64, :], asg[0:64, :], tab[0:64, 9:10], om[0:64, 6:7], ALU.mult, ALU.add, reads=[asgb, tabb, omb], writes=[kmb])
                cx.tt("pool", km[0:64, :], km[0:64, :], k0[0:64, :], ALU.mult, reads=[kmb, k0b], writes=[kmb])
                rkr, rkrb = T("rw_rkr")
                cx.stt("dve", rkr[0:64, :], r[0:64, :], tab[0:64, 10:11], km[0:64, :], ALU.mult, ALU.mult, reads=[rb_, tabb, kmb], writes=[rkrb])
                AR, ARb = T("rw_AR", 1024)
                AR4 = AR[0:64, :].rearrange("p (c two t) -> p c two t", two=2, t=64)
                cx.stt("dve", AR4[:, :, 0, :], v3(kks, 64), -1.0, v3(ge, 64), ALU.mult, ALU.mult, reads=[kksb, geb], writes=[ARb])
                cx.tt("pool", AR4[:, :, 1, :], v3(r, 64), v3(gi, 64), ALU.mult, reads=[rb_, gib], writes=[ARb])
                Bt, Btb = T("rw_Bt"); Kt, Ktb = T("rw_Kt")
                cx.tt("dve", Bt[0:64, :], kks[0:64, :], asg[0:64, :], ALU.mult, reads=[kksb, asgb], writes=[Btb])
                cx.tt("dve", Bt[0:64, :], Bt[0:64, :], gv[0:64, :], ALU.mult, reads=[Btb, gvb], writes=[Btb])
                cx.tt("pool", Kt[0:64, :], km[0:64, :], gv[0:64, :], ALU.mult, reads=[kmb, gvb], writes=[Ktb])
                WA, WAb = T("rw_WA", 1024)
                WA3 = v3(WA, 128)
                BT, BTb = T("rw_BT"); KT, KTb = T("rw_KT"); VT, VTb = T("rw_VT")
                BT3, KT3, VT3 = v3(BT, 64), v3(KT, 64), v3(VT, 64)
                for (src, srcb, dst3, dstb, strided) in ((None, ARb, WA3[:, :, 64:128], WAb, True), (Bt, Btb, BT3, BTb, False),
                                                         (Kt, Ktb, KT3, KTb, False), (v, vb, VT3, VTb, False)):
                    pt, ptb = psC.next()
                    for c in range(8):
                        lhs = AR4[:, c, 0, :] if strided else src[0:64, c * 64:(c + 1) * 64]
                        cx.mm(pt[0:64, c * 64:(c + 1) * 64], lhs, cst[0:64, 0:64], True, True, reads=[srcb, cstb], writes=[ptb])
                    cx.cp("dve", dst3, v3(pt, 64), reads=[ptb], writes=[dstb])
                E1, E1b = T("rw_E1", 1024); E2, E2b = T("rw_E2", 1024)
                E13, E23 = v3(E1, 128), v3(E2, 128)
                for (lt, ltb, E, Eb) in ((Bt, Btb, E1, E1b), (Kt, Ktb, E2, E2b)):
                    pp, ppb = psB.next()
                    for c in range(8):
                        cx.mm(pp[0:64, c * 128:(c + 1) * 128], lt[0:64, c * 64:(c + 1) * 64], AR4[:, c, :, :], True, True,
                              reads=[ltb, ARb], writes=[ppb])
                    for half in range(2):
                        cx.tt("dve", E[0:64, half * 512:(half + 1) * 512], pp[0:64, half * 512:(half + 1) * 512],
                              mm64x4[:].rearrange("p c x -> p (c x)"), ALU.mult, reads=[ppb, repb], writes=[Eb])
                XT, XTb = T("rw_XT")
                px, pxb = psC.next()
                for c in range(8):
                    cx.mm(px[0:64, c * 64:(c + 1) * 64], AR4[:, c, 0, :], Bt[0:64, c * 64:(c + 1) * 64], True, True,
                          reads=[ARb, Btb], writes=[pxb])
                cx.tt("dve", XT[0:64, :], px[0:64, :], ml64x8[:].rearrange("p c x -> p (c x)"), ALU.mult, reads=[pxb, repb], writes=[XTb])
                Sm, Smb = T("rw_Sm")
                Sm3 = v3(Sm, 64)
                cx.tt("dve", Sm3, E13[:, :, 0:64], i64x8[:], ALU.add, reads=[E1b, repb], writes=[Smb])
                Pc3, Pcb = E13[:, :, 0:64], E1b
                PT3, PTb = v3(XT, 64), XTb
                for lvl in range(5):
                    last = lvl == 4
                    if not last:
                        pa_, pab_ = psC.next()
                        for c in range(8):
                            cx.mm(pa_[0:64, c * 64:(c + 1) * 64], PT3[:, c, :], Pc3[:, c, :], True, True, reads=[PTb, Pcb], writes=[pab_])
                    pb2, pb2b = psC.next()
                    for c in range(8):
                        cx.mm(pb2[0:64, c * 64:(c + 1) * 64], Pc3[:, c, :], PT3[:, c, :], True, True, reads=[PTb, Pcb], writes=[pb2b])
                    if not last:
                        Pn, Pnb = T(f"rw_P{lvl % 2}")
                        cx.cp("dve", Pn[0:64, :], pa_[0:64, :], reads=[pab_], writes=[Pnb])
                    PTn, PTnb = T(f"rw_PT{lvl % 2}")
                    cx.cp("dve", PTn[0:64, :], pb2[0:64, :], reads=[pb2b], writes=[PTnb])
                    if not last:
                        Pc3, Pcb = v3(Pn, 64), Pnb
                    PT3, PTb = v3(PTn, 64), PTnb
                    pc_, pcb_ = psC.next()
                    for c in range(8):
                        cx.mm(pc_[0:64, c * 64:(c + 1) * 64], PT3[:, c, :], Sm3[:, c, :], True, True, reads=[PTb, Smb], writes=[pcb_])
                    cx.tt("dve", Sm[0:64, :], Sm[0:64, :], pc_[0:64, :], ALU.add, reads=[Smb, pcb_], writes=[Smb])
                pw, pwb = psC.next()
                for c in range(8):
                    cx.mm(pw[0:64, c * 64:(c + 1) * 64], E23[:, c, 0:64], VT3[:, c, :], True, True, reads=[E2b, VTb], writes=[pwb])
                cx.cp("dve", WA3[:, :, 0:64], v3(pw, 64), reads=[pwb], writes=[WAb])
                UA, UAb = T("rw_UA", 1024)
                UA3 = v3(UA, 128)
                pu, pub = psB.next()
                for c in range(8):
                    cx.mm(pu[0:64, c * 128:(c + 1) * 128], Sm3[:, c, :], WA3[:, c, :], True, True, reads=[Smb, WAb], writes=[pub])
                for half in range(2):
                    cx.cp("dve", UA[0:64, half * 512:(half + 1) * 512], pu[0:64, half * 512:(half + 1) * 512], reads=[pub], writes=[UAb])
                M0, M0b = T("rw_M0"); Qe, Qeb = T("rw_Qe")
                M03, Qe3 = v3(M0, 64), v3(Qe, 64)
                pm_, pmb_ = psC.next()
                for c in range(8):
                    cx.mm(pm_[0:64, c * 64:(c + 1) * 64], UA3[:, c, 64:128], BT3[:, c, :], True, True, reads=[UAb, BTb], writes=[pmb_])
                cx.tt("dve", M03, v3(pm_, 64), i64x8[:], ALU.add, reads=[pmb_, repb], writes=[M0b])
                pq, pqb = psC.next()
                for c in range(8):
                    cx.mm(pq[0:64, c * 64:(c + 1) * 64], UA3[:, c, 64:128], E13[:, c, 64:128], True, True, reads=[UAb, E1b], writes=[pqb])
                cx.tt("dve", Qe3, v3(pq, 64), AR4[:, :, 1, :], ALU.add, reads=[pqb, ARb], writes=[Qeb])
                pbn, pbnb = psC.next()
                for c in range(8):
                    cx.mm(pbn[0:64, c:c + 1], rkr[0:64, c * 64:(c + 1) * 64], ones[0:64, 0:1], True, True, reads=[rkrb, cstb], writes=[pbnb])
                bsm, bsmb = sm.next()
                cx.cp("dve", bsm[0:64, 0:8], pbn[0:64, 0:8], reads=[pbnb], writes=[bsmb])
                pgt, pgtb = psC.next()
                for c in range(8):
                    cx.mm(pgt[0:64, c * 64:(c + 1) * 64], sgd[:, c * 64:(c + 1) * 64], rwg2[s][0][:], True, True,
                          reads=[sgdb, rwg2[s][1]], writes=[pgtb])
                gT, gTb = T("rw_gT")
                cx.cp("act", gT[0:64, :], pgt[0:64, :], reads=[pgtb], writes=[gTb])
                py, pyb = psA.next()
                for c in range(8):
                    Hc, Hcb = rwH[s][hidx[s]]
                    Hn, Hnb = rwH[s][1 - hidx[s]]
                    hidx[s] = 1 - hidx[s]
                    ysl = py[0:64, c * 64:(c + 1) * 64]
                    cx.mm(ysl, E13[:, c, 64:128], UA3[:, c, 0:64], True, False, reads=[E1b, UAb], writes=[pyb])
                    cx.mm(ysl, E23[:, c, 64:128], VT3[:, c, :], False, False, reads=[E2b, VTb], writes=[pyb])
                    cx.mm(ysl, Qe3[:, c, :], Hc[:], False, True, reads=[Qeb, Hcb], writes=[pyb])
                    pxs, pxsb = psC.next()
                    cx.mm(pxs[0:64, 0:64], BT3[:, c, :], UA3[:, c, 0:64], True, False, reads=[BTb, UAb], writes=[pxsb])
                    cx.mm(pxs[0:64, 0:64], KT3[:, c, :], VT3[:, c, :], False, False, reads=[KTb, VTb], writes=[pxsb])
                    cx.mm(pxs[0:64, 0:64], M03[:, c, :], Hc[:], False, True, reads=[M0b, Hcb], writes=[pxsb])
                    cx.ts("dve", Hn[:], pxs[0:64, 0:64], gL[0:64, c:c + 1], None, ALU.mult, reads=[pxsb, gLb], writes=[Hnb])
                yr, yrb = T("rw_yr"); yn, ynb = T("rw_yn")
                cx.cp("dve", yr[0:64, :], py[0:64, :], reads=[pyb], writes=[yrb])
                yr3, yn3 = v3(yr, 64), v3(yn, 64)
                groupnorm(yr3, yrb, 64, 8, GN_EPS, yn3, ynb)
                cx.tt("pool", yn3, yn3, gbt[:, 0, :, :], ALU.mult, reads=[ynb, gbtb], writes=[ynb])
                cx.tt("pool", yn3, yn3, gbt[:, 1, :, :], ALU.add, reads=[ynb, gbtb], writes=[ynb])
                cx.tt("dve", yr3, VT3, bc3(bsm[0:64, 0:8], 64), ALU.mult, reads=[VTb, bsmb], writes=[yrb])
                cx.tt("dve", yn3, yn3, yr3, ALU.add, reads=[ynb, yrb], writes=[ynb])
                o, ob = oring.next()
                cx.tt("dve", o[0:64, :], yn[0:64, :], gT[0:64, :], ALU.mult, reads=[ynb, gTb], writes=[ob])
                cx.dma(y_rw[s][t0:t0 + TB, :].rearrange("(c t) v -> t c v", t=64), v3(o, 64), reads=[ob], writes=[Buf()])
        cx.finish()
    return nc


POOL_W = 256
RWKV_W = 384
RW_BASE = POOL_W
ML_BASE = POOL_W + 3 * RWKV_W + 256
WINS = (2, 4, 8, 16)


def make_consts():
    c = np.zeros((128, 1024), np.float32)
    i = np.arange(128)
    c[:, 0:128] = np.eye(128)
    c[:, 128:256] = (i[:, None] <= i[None, :])
    c[:, 256:384] = (i[:, None] <= i[None, :])
    j = np.arange(64)
    c[0:64, 384:448] = (j[None, :] > j[:, None])
    c[0:64, 448:512] = (j[None, :] >= j[:, None])
    c[0:64, 512:576] = (j[None, :] < j[:, None])
    c[:, 576:640] = 1.0
    same = (i[:, None] // 64) == (i[None, :] // 64)
    c[:, 640:768] = same & (i[:, None] <= i[None, :])
    c[:, 768:896] = same & (i[:, None] < i[None, :])
    return c


def slot_heads(j):
    return [2 * j, 2 * j + 1] if j < 3 else [0, 1]


def m_in_map(inp, l, hT_b, j, consts, vfirst_b=None):
    w_in = inp["w_in"][l]
    m = {"hT": hT_b, "consts": consts}
    for s, h in enumerate(slot_heads(j)):
        hc = slice(h * 64, (h + 1) * 64)
        cols = np.concatenate([RW_BASE + np.arange(h * 64, (h + 1) * 64), RW_BASE + 384 + np.arange(h * 64, (h + 1) * 64),
                               RW_BASE + 768 + np.arange(h * 64, (h + 1) * 64), RW_BASE + 1152 + np.arange(256)])
        m[f"rw_w{s}"] = np.ascontiguousarray(w_in[:, cols])
        mu = inp["rwkv_mu"][l][cols - RW_BASE]
        tab = np.zeros((128, 16), np.float32)
        for g in range(5):
            tab[0:64, g] = mu[g * 64:(g + 1) * 64]
        tab[:, 5] = mu[320:448]
        tab[0:64, 6] = inp["rwkv_w0"][l][hc]
        tab[0:64, 7] = inp["rwkv_a0"][l][hc]
        tab[0:64, 8] = inp["rwkv_kk_scale"][l][hc]
        tab[0:64, 9] = inp["rwkv_ka"][l][hc]
        tab[0:64, 10] = inp["rwkv_rk"][l][h]
        if l > 0:
            tab[0:64, 11] = inp["rwkv_v0"][l - 1][hc]
        m[f"rw_tab{s}"] = tab
        m[f"rw_w2{s}"] = np.ascontiguousarray(inp["rwkv_w2"][l][:, hc])
        m[f"rw_a2{s}"] = np.ascontiguousarray(inp["rwkv_a2"][l][:, hc])
        m[f"rw_g2{s}"] = np.ascontiguousarray(inp["rwkv_g2"][l][:, hc])
        m[f"rw_rows{s}"] = np.stack([inp["rwkv_w0"][l][hc], inp["rwkv_lnx_g"][l][hc], inp["rwkv_lnx_b"][l][hc]]).astype(np.float32)
        if l > 0:
            m[f"rw_v2{s}"] = np.ascontiguousarray(inp["rwkv_v2"][l - 1][:, hc])
            m[f"rw_vf{s}"] = np.ascontiguousarray(vfirst_b[h])
        qc = ML_BASE + np.arange(h * 64, (h + 1) * 64)
        m[f"ml_wqk{s}"] = np.ascontiguousarray(w_in[:, np.concatenate([qc, qc + 384])])
        m[f"ml_wvog{s}"] = np.ascontiguousarray(w_in[:, np.concatenate([qc + 768, qc + 1152, [ML_BASE + 1536 + h, ML_BASE + 1542 + h]])])
        mt = np.zeros((64, 16), np.float32)
        cw = inp["mlstm_conv_w"][l]; cb = inp["mlstm_conv_b"][l]
        for g in range(2):
            ch = slice(g * 384 + h * 64, g * 384 + (h + 1) * 64)
            for i in range(4):
                mt[:, g * 5 + i] = cw[i, ch]
            mt[:, g * 5 + 4] = cb[ch]
        m[f"ml_tab{s}"] = mt
        m[f"ml_rows{s}"] = np.stack([inp["mlstm_norm_g"][l][hc], np.full(64, inp["mlstm_b_i"][l][h], np.float32),
                                     np.full(64, inp["mlstm_b_f"][l][h], np.float32)]).astype(np.float32)
    if l > 0:
        m["rw_v1"] = inp["rwkv_v1"][l - 1]
    gi = j
    win = WINS[gi]
    m["pl_w"] = np.ascontiguousarray(w_in[:, gi * 64:(gi + 1) * 64])
    m["pl_pw"] = inp["pool_w"][l][gi]
    pt = np.zeros((64, 40), np.float32)
    pt[:, 0:win] = 1.0
    pt[:, 16] = 1.0 / win
    pt[:, 17] = inp["pool_scale"][l][gi * 64:(gi + 1) * 64]
    pt[:, 20:36] = 1.0 / np.minimum(np.arange(16) + 1, win)[None, :]
    m["pl_tab"] = pt
    return m


_PROGS = {}


def _prog(name, fn):
    if name not in _PROGS:
        _PROGS[name] = fn()
    return _PROGS[name]


F16 = mybir.dt.float16
CAP = 384


def make_fconsts():
    c = np.zeros((128, 1024), np.float32)
    i = np.arange(128)
    c[:, 0:128] = np.eye(128)
    c[:, 128:256] = (i[:, None] < i[None, :])
    c[:, 256:384] = 1.0
    c[:, 384:768] = np.arange(CAP)[None, :]
    for k in range(3):
        c[:, 768 + k] = i + 128 * k
    for e in range(NE):
        pass
    return c


def build_F2(n_exp=NE, final=True):
    from contextlib import ExitStack
    nc = bass.Bass("TRN2", target_bir_lowering=False)
    dr = lambda n, s, k="ExternalInput": nc.dram_tensor(n, list(s), F32, kind=k).ap()
    yT = dr("yT", [D, NT_F])
    hin = dr("hin", [NT_F, D])
    w_out = dr("w_out", [D, D])
    ln1g = dr("ln1g", [1, D]); ln1b = dr("ln1b", [1, D])
    ln2g = dr("ln2g", [1, D]); ln2b = dr("ln2b", [1, D])
    router_w = dr("router_w", [D, NE]); router_b = dr("router_b", [1, NE])
    nE_w = max(1, n_exp)
    w_gu = dr("w_gu", [nE_w, D, 2 * D]); b_gu = dr("b_gu", [NE, 2 * D])
    w_dn = dr("w_dn", [nE_w, D, D]); b_dn = dr("b_dn", [NE, D])
    fconsts = dr("fconsts", [128, 1024])
    esel_d = dr("esel", [NE, NE * 128])
    out = dr("out", [NT_F, D], "ExternalOutput")

    with ExitStack() as stack:
        cx = Ctx(nc, stack)
        NTILE = NT_F // 128
        cst = cx.sb([128, 1024]); cstb = Buf()
        cx.dma(cst[:], fconsts[:, :], writes=[cstb])
        ident = cst[:, 0:128]; identb = cstb
        triS = cst[:, 128:256]
        ones = cst[:, 256:384]
        iota_r = cst[:, 384:768]
        h1T = cx.sb([128, NTILE, D], BF16); h1Tb = [Buf() for _ in range(NTILE)]
        yacc = cx.sb([128, NTILE, D]); yaccb = [Buf() for _ in range(NTILE)]
        MK = cx.sb([128, NTILE, NE]); MKb = [Buf() for _ in range(NTILE)]
        PS_ = cx.sb([128, NTILE, NE]); PSb = [Buf() for _ in range(NTILE)]
        GF = cx.sb([NE, NT_F], F16); PF = cx.sb([NE, NT_F], F16); GPFb = [Buf() for _ in range(NTILE)]
        bguT = cx.sb([128, 16, NE]); bguTb = Buf()
        esel = cx.sb([NE, NE * 128], F16); eselb = Buf()

        with ExitStack() as sa:
            ca = Ctx(nc, sa); ca.S = cx.S; ca.n = 1000
            wo = ca.sb([128, 8, D], BF16); wob = Buf()
            g1 = (ca.sb([128, D]), Buf()); b1 = (ca.sb([128, D]), Buf())
            rw = ca.sb([128, 8, NE]); rwb = Buf()
            rb = ca.sb([128, NE]); rbb = Buf()
            bdn = ca.sb([NE, D]); bdnb = Buf()
            cnt = ca.sb([128, NE]); cntb = Buf()
            G = ca.sb([128, NTILE, NE]); Gb = [Buf() for _ in range(NTILE)]
            ca.memset("pool", cnt[:], 0.0, writes=[cntb])
            ca.dma(bdn[:], b_dn[:, :], writes=[bdnb])
            sa0 = ExitStack(); ca0 = Ctx(nc, sa0); ca0.S = cx.S; ca0.n = 500
            bgu_s = ca0.sb([NE, 2 * D]); bgu_sb = Buf()
            stage = Ring(ca0, 2, [128, 2, D])
            es_st = ca0.sb([NE, NE * 128]); es_stb = Buf()
            pst = Ring(ca, 2, [128, 512], psum=True)
            ca.dma(es_st[:], esel_d[:, :], writes=[es_stb])
            ca.cp("dve", esel[:], es_st[:], reads=[es_stb], writes=[eselb])
            ca.dma(g1[0][:], ln1g.partition_broadcast(128), writes=[g1[1]])
            ca.dma(b1[0][:], ln1b.partition_broadcast(128), writes=[b1[1]])
            ca.dma(rb[:], router_b.partition_broadcast(128), writes=[rbb])
            ca.dma(rw[:], router_w.rearrange("(kc p) n -> p kc n", p=128), writes=[rwb])
            ca.dma(bgu_s[:], b_gu[:, :], writes=[bgu_sb])
            wo_v = w_out.rearrange("(kc p) n -> p kc n", p=128)
            for hh in range(4):
                st_t, st_b = stage.next()
                ca.dma(st_t[:], wo_v[:, hh * 2:(hh + 1) * 2, :], writes=[st_b])
                ca.cp("pool", wo[:, hh * 2:(hh + 1) * 2, :], st_t[:], reads=[st_b], writes=[wob])
            for q in range(4):
                pt, pb = pst.next()
                for jj in range(4):
                    j = q * 4 + jj
                    ca.tr(pt[:, jj * NE:(jj + 1) * NE], bgu_s[:, j * 128:(j + 1) * 128], ident[0:NE, 0:NE],
                          reads=[bgu_sb, identb], writes=[pb])
                ca.cp("dve", bguT[:, q * 4:(q + 1) * 4, :], pt[:, 0:4 * NE].rearrange("p (j e) -> p j e", e=NE),
                      reads=[pb], writes=[bguTb])
            sa0.close()
            cx.S.barrier()
            yst = Ring(ca, 2, [128, 8, 128])
            ybf = Ring(ca, 2, [128, 8, 128], BF16)
            hring = Ring(ca, 2, [128, D])
            zring = Ring(ca, 1, [128, D])
            h1ring = Ring(ca, 2, [128, D])
            tmpring = Ring(ca, 2, [128, D])
            stat = Ring(ca, 4, [128, 8])
            h1f32 = Ring(ca, 1, [128, 8, 128])
            psm = Ring(ca, 2, [128, 1024], psum=True)
            psr = Ring(ca, 2, [128, 512], psum=True)
            lg_r = Ring(ca, 2, [128, NE]); ex_r = Ring(ca, 2, [128, NE]); t8_r = Ring(ca, 2, [128, 8])
            gf_r = Ring(ca, 2, [NE, 128])
            yT_v = yT.rearrange("(kc p) t -> p kc t", p=128)
            for tb in range(NT_F // 128):
                ys, ysb = yst.next()
                ca.dma(ys[:], yT_v[:, :, tb * 128:(tb + 1) * 128], writes=[ysb])
                yb, ybb = ybf.next()
                ca.cp("pool", yb[:], ys[:], reads=[ysb], writes=[ybb])
                for ti in range(1):
                    i = tb
                    ht, hb = hring.next()
                    ca.dma(ht[:], hin[i * 128:(i + 1) * 128, :], writes=[hb])
                    pm, pmb = psm.next()
                    for half in range(2):
                        for kc in range(8):
                            ca.mm(pm[:, half * 512:(half + 1) * 512], yb[:, kc, ti * 128:(ti + 1) * 128],
                                  wo[:, kc, half * 512:(half + 1) * 512], kc == 0, kc == 7,
                                  reads=[ybb, wob], writes=[pmb])
                    z, zb = zring.next()
                    for half in range(2):
                        hs = slice(half * 512, (half + 1) * 512)
                        ca.stt("dve", z[:, hs], ht[:, hs], ALPHA, pm[:, hs], ALU.mult, ALU.add, reads=[hb, pmb], writes=[zb])
                    h1, h1b = h1ring.next()
                    layer_norm_tile(ca, z[:], zb, h1[:], h1b, g1, b1, tmpring, stat)
                    ca.cp("act", h1T[:, i, :], h1[:], reads=[h1b], writes=[h1Tb[i]])
                    hf, hfb = h1f32.next()
                    for q in range(2):
                        pt, pb = pst.next()
                        for jj in range(4):
                            kc = q * 4 + jj
                            ca.tr(pt[:, jj * 128:(jj + 1) * 128], h1[:, kc * 128:(kc + 1) * 128], ident,
                                  reads=[h1b, identb], writes=[pb])
                        pv = pt[:].rearrange("p (j t) -> p j t", t=128)
                        ca.cp("dve", hf[:, q * 4:(q + 1) * 4, :], pv, reads=[pb], writes=[hfb])
                    pr, prb = psr.next()
                    for kc in range(8):
                        ca.mm(pr[:, 0:NE], hf[:, kc, :], rw[:, kc, :], kc == 0, kc == 7, reads=[hfb, rwb], writes=[prb])
                    lg, lgb = lg_r.next(); ex, exb = ex_r.next(); t8, t8b = t8_r.next()
                    ca.tt("dve", lg[:], pr[:, 0:NE], rb[:], ALU.add, reads=[prb, rbb], writes=[lgb])
                    ca.S.add("dve", lambda h, o=t8, a=lg: h.max(out=o[:], in_=a[:]), [lgb], [t8b])
                    ca.ts("dve", MK[:, i, :], lg[:], t8[:, 3:4], None, ALU.is_ge, reads=[lgb, t8b], writes=[MKb[i]])
                    ca.ts("dve", t8[:, 4:5], t8[:, 0:1], -1.0, None, ALU.mult, reads=[t8b], writes=[t8b])
                    ca.act(lg[:], lg[:], AF.Exp, bias=t8[:, 4:5], scale=1.0, reads=[lgb, t8b], writes=[lgb])
                    ca.tt("dve", ex[:], MK[:, i, :], lg[:], ALU.mult, reads=[MKb[i], lgb], writes=[exb])
                    ca.rsum("dve", t8[:, 5:6], ex[:], reads=[exb], writes=[t8b])
                    ca.recip(t8[:, 6:7], t8[:, 5:6], reads=[t8b], writes=[t8b])
                    ca.ts("dve", G[:, i, :], ex[:], t8[:, 6:7], None, ALU.mult, reads=[exb, t8b], writes=[Gb[i]])
                    pp, ppb = psr.next()
                    ca.mm(pp[:, 0:NE], triS, MK[:, i, :], True, True, reads=[cstb, MKb[i]], writes=[ppb])
                    ca.mm(pp[:, NE:2 * NE], ones, MK[:, i, :], True, True, reads=[cstb, MKb[i]], writes=[ppb])
                    ca.tt("dve", PS_[:, i, :], pp[:, 0:NE], cnt[:], ALU.add, reads=[ppb, cntb], writes=[PSb[i]])
                    ca.tt("dve", cnt[:], cnt[:], pp[:, NE:2 * NE], ALU.add, reads=[ppb, cntb], writes=[cntb])
                    pr2, pr2b = psr.next()
                    ca.tr(pr2[0:NE, 0:128], G[:, i, :], ident, reads=[Gb[i], identb], writes=[pr2b])
                    ca.tr(pr2[0:NE, 128:256], PS_[:, i, :], ident, reads=[PSb[i], identb], writes=[pr2b])
                    gf, gfb = gf_r.next()
                    ca.cp("dve", gf[:], pr2[0:NE, 0:128], reads=[pr2b], writes=[gfb])
                    ca.cp("pool", GF[:, i * 128:(i + 1) * 128], gf[:], reads=[gfb], writes=[GPFb[i]])
                    ca.cp("dve", PF[:, i * 128:(i + 1) * 128], pr2[0:NE, 128:256], reads=[pr2b], writes=[GPFb[i]])
                    pm2, pm2b = psm.next()
                    for half in range(2):
                        ca.mm(pm2[:, half * 512:(half + 1) * 512], gf[:], bdn[:, half * 512:(half + 1) * 512], True, True,
                              reads=[gfb, bdnb], writes=[pm2b])
                    for half in range(2):
                        hs = slice(half * 512, (half + 1) * 512)
                        ca.stt("dve", yacc[:, i, hs], h1[:, hs], ALPHA, pm2[:, hs], ALU.mult, ALU.add,
                               reads=[h1b, pm2b], writes=[yaccb[i]])

        cx.S.barrier()
        with ExitStack() as sb_:
            cb = Ctx(nc, sb_); cb.S = cx.S; cb.n = 2000
            stage = Ring(cb, 2, [128, 1024])
            wg_r = Ring(cb, 2, [128, 8, 512], BF16)
            wl_r = Ring(cb, 2, [128, 8, 512], BF16)
            wd_r = Ring(cb, 2, [128, 4, D], BF16)
            sel_r = Ring(cb, 2, [128, CAP], BF16)
            xe_r = Ring(cb, 1, [128, 8, CAP], BF16)
            actr = Ring(cb, 1, [128, 4, CAP], BF16)
            oute_r = Ring(cb, 1, [128, 3, D], BF16)
            sgt_r = Ring(cb, 1, [128, 3, 512], BF16)
            gbc_r = Ring(cb, 1, [128, 512])
            glu_r = Ring(cb, 2, [128, CAP]); sig_r = Ring(cb, 1, [128, CAP]); lin_r = Ring(cb, 1, [128, CAP])
            pA = Ring(cb, 4, [128, 512], psum=True)
            pB = Ring(cb, 4, [128, 512], psum=True)
            all_h1T = h1Tb
            for e in range(n_exp):
                gu_v = w_gu[e].rearrange("(kc p) n -> p kc n", p=128)
                dn_v = w_dn[e].rearrange("(j p) n -> p j n", p=128)
                xe, xeb = xe_r.next()
                for grp in range(2):
                    accs = [pA.next() for _ in range(4)]
                    for i in range(NTILE):
                        sl, slb = sel_r.next()
                        cb.ts("dve", sl[:], iota_r, PS_[:, i, e:e + 1], MK[:, i, e:e + 1], ALU.is_equal, ALU.mult,
                              reads=[cstb, PSb[i], MKb[i]], writes=[slb])
                        for q in range(4):
                            kc = grp * 4 + q
                            cb.mm(accs[q][0][:, 0:CAP], h1T[:, i, kc * 128:(kc + 1) * 128], sl[:], i == 0, i == NTILE - 1,
                                  reads=[h1Tb[i], slb], writes=[accs[q][1]])
                    for q in range(4):
                        kc = grp * 4 + q
                        cb.cp("act" if q % 2 else "dve", xe[:, kc, :], accs[q][0][:, 0:CAP], reads=[accs[q][1]], writes=[xeb])
                for hf_ in range(2):
                    wg, wgb = wg_r.next(); wl, wlb = wl_r.next(); wd, wdb = wd_r.next()
                    for (dst, dstb, c0) in ((wg, wgb, hf_ * 512), (wl, wlb, D + hf_ * 512)):
                        for q in range(4):
                            st_t, st_b = stage.next()
                            cb.dma(st_t[:].rearrange("p (k n) -> p k n", n=512), gu_v[:, q * 2:(q + 1) * 2, c0:c0 + 512], writes=[st_b])
                            cb.cp("pool", dst[:, q * 2:(q + 1) * 2, :], st_t[:].rearrange("p (k n) -> p k n", n=512),
                                  reads=[st_b], writes=[dstb])
                    for q in range(4):
                        st_t, st_b = stage.next()
                        cb.dma(st_t[:], dn_v[:, hf_ * 4 + q, :], writes=[st_b])
                        cb.cp("act", wd[:, q, :], st_t[:], reads=[st_b], writes=[wdb])
                    ac, acb = actr.next()
                    for j in range(4):
                        pg, pgb = pA.next(); pl, plb = pA.next()
                        for kc in range(8):
                            cb.mm(pg[:, 0:CAP], wg[:, kc, j * 128:(j + 1) * 128], xe[:, kc, :], kc == 0, kc == 7,
                                  reads=[wgb, xeb], writes=[pgb])
                        for kc in range(8):
                            cb.mm(pl[:, 0:CAP], wl[:, kc, j * 128:(j + 1) * 128], xe[:, kc, :], kc == 0, kc == 7,
                                  reads=[wlb, xeb], writes=[plb])
                        jg = hf_ * 4 + j
                        glu, glub = glu_r.next(); sig, sigb = sig_r.next(); lin, linb = lin_r.next()
                        cb.ts("dve", glu[:], pg[:, 0:CAP], bguT[:, jg, e:e + 1], 7.0, ALU.add, ALU.min, reads=[pgb, bguTb], writes=[glub])
                        cb.act(sig[:], glu[:], AF.Sigmoid, scale=1.702, reads=[glub], writes=[sigb])
                        cb.ts("dve", lin[:], pl[:, 0:CAP], bguT[:, 8 + jg, e:e + 1], 7.0, ALU.add, ALU.min, reads=[plb, bguTb], writes=[linb])
                        cb.ts("pool", lin[:], lin[:], -7.0, 1.0, ALU.max, ALU.add, reads=[linb], writes=[linb])
                        cb.tt("pool", glu[:], glu[:], sig[:], ALU.mult, reads=[glub, sigb], writes=[glub])
                        cb.tt("dve", ac[:, j, :], glu[:], lin[:], ALU.mult, reads=[glub, linb], writes=[acb])
                    oe, oeb = oute_r.next()
                    for st in range(3):
                        for half in range(2):
                            po, pob = pB.next()
                            for j in range(4):
                                cb.mm(po[:], ac[:, j, st * 128:(st + 1) * 128], wd[:, j, half * 512:(half + 1) * 512],
                                      j == 0, j == 3, reads=[acb, wdb], writes=[pob])
                            cb.cp("act" if half else "dve", oe[:, st, half * 512:(half + 1) * 512], po[:], reads=[pob], writes=[oeb])
                    for tb in range(NT_F // 512):
                        tsl = slice(tb * 512, (tb + 1) * 512)
                        rd = [GPFb[tb * 4 + q] for q in range(4)]
                        pbp, pbpb = pB.next(); pbg, pbgb = pB.next()
                        cb.mm(pbp[:], esel[:, e * 128:(e + 1) * 128], PF[:, tsl], True, True, reads=[eselb] + rd, writes=[pbpb])
                        cb.mm(pbg[:], esel[:, e * 128:(e + 1) * 128], GF[:, tsl], True, True, reads=[eselb] + rd, writes=[pbgb])
                        gbc, gbcb = gbc_r.next()
                        cb.cp("act", gbc[:], pbg[:], reads=[pbgb], writes=[gbcb])
                        sg, sgb = sgt_r.next()
                        for st in range(3):
                            cb.stt("dve", sg[:, st, :], pbp[:], cst[:, 768 + st:769 + st], gbc[:], ALU.is_equal, ALU.mult,
                                   reads=[pbpb, cstb, gbcb], writes=[sgb])
                        for ti in range(4):
                            i = tb * 4 + ti
                            for half in range(2):
                                ps2, ps2b = pB.next()
                                for st in range(3):
                                    cb.mm(ps2[:], sg[:, st, ti * 128:(ti + 1) * 128], oe[:, st, half * 512:(half + 1) * 512],
                                          st == 0, st == 2, reads=[sgb, oeb], writes=[ps2b])
                                ysl = yacc[:, i, half * 512:(half + 1) * 512]
                                cb.tt("dve", ysl, ysl, ps2[:], ALU.add, reads=[ps2b, yaccb[i]], writes=[yaccb[i]])

        cx.S.barrier()
        with ExitStack() as sc:
            cc = Ctx(nc, sc); cc.S = cx.S; cc.n = 3000
            g2 = (cc.sb([128, D]), Buf()); b2 = (cc.sb([128, D]), Buf())
            cc.dma(g2[0][:], ln2g.partition_broadcast(128), writes=[g2[1]])
            cc.dma(b2[0][:], ln2b.partition_broadcast(128), writes=[b2[1]])
            tmpring = Ring(cc, 2, [128, D]); stat = Ring(cc, 4, [128, 8]); oring = Ring(cc, 3, [128, D])
            outb = Buf()
            for i in range(NTILE):
                o, ob = oring.next()
                layer_norm_tile(cc, yacc[:, i, :], yaccb[i], o[:], ob, g2, b2, tmpring, stat)
                cc.dma(out[i * 128:(i + 1) * 128, :], o[:], reads=[ob], writes=[outb])
        cx.finish()
    return nc


def make_esel():
    es = np.zeros((NE, NE * 128), np.float32)
    for e in range(NE):
        es[e, e * 128:(e + 1) * 128] = 1.0
    return es


def kernel(**inputs):
    inp = {k: np.asarray(v) for k, v in inputs.items()}
    x = inp["x"].astype(np.float32)
    B, S, _ = x.shape
    consts = make_consts()
    fconsts = make_fconsts(); esel = make_esel()
    h = x
    vfirst = None
    for l in range(2):
        ncM = _prog(f"M{l}", lambda: build_M(l > 0))
        hT = [np.ascontiguousarray(h[b].T) for b in range(B)]
        maps = []
        for c in range(NCORES):
            b, j = c // 4, c % 4
            maps.append(m_in_map(inp, l, hT[b], j, consts, None if l == 0 else vfirst[b]))
        res = run_bass_kernel_spmd(ncM, maps, core_ids=list(range(NCORES))).results
        ycat = np.zeros((B, S, D), np.float32)
        if l == 0:
            vfirst = [[None] * 6 for _ in range(B)]
        for c in range(NCORES):
            b, j = c // 4, c % 4
            ycat[b, :, j * 64:(j + 1) * 64] = res[c]["y_pl"].T
            if j < 3:
                for s, hd in enumerate(slot_heads(j)):
                    ycat[b, :, POOL_W + hd * 64:POOL_W + (hd + 1) * 64] = res[c][f"y_rw{s}"]
                    ycat[b, :, POOL_W + RWKV_W + hd * 64:POOL_W + RWKV_W + (hd + 1) * 64] = res[c][f"y_ml{s}"]
                    if l == 0:
                        vfirst[b][hd] = np.ascontiguousarray(res[c][f"vf_out{s}"])
        ncF = _prog("F2", build_F2)
        ycf = ycat.reshape(B * S, D)
        hf = h.reshape(B * S, D)
        mapsF = []
        for c in range(NCORES):
            sl = slice(c * NT_F, (c + 1) * NT_F)
            mapsF.append(dict(
                yT=np.ascontiguousarray(ycf[sl].T), hin=np.ascontiguousarray(hf[sl]), w_out=inp["w_out"][l],
                ln1g=inp["ln1_g"][l][None], ln1b=inp["ln1_b"][l][None], ln2g=inp["ln2_g"][l][None], ln2b=inp["ln2_b"][l][None],
                router_w=inp["router_w"][l], router_b=inp["router_b"][l][None], w_gu=inp["w_gate_up"][l], b_gu=inp["b_gate_up"][l],
                w_dn=inp["w_down"][l], b_dn=inp["b_down"][l], fconsts=fconsts, esel=esel))
        resF = run_bass_kernel_spmd(ncF, mapsF, core_ids=list(range(NCORES))).results
        h = np.concatenate([resF[c]["out"] for c in range(NCORES)], 0).reshape(B, S, D)
    return h.astype(np.float32)
```
